# Optimizing a Trainium2 kernel written in Bass

```python
import jax, jax.numpy as jnp
from jax import lax
import numpy as np

D_MODEL = 1024
BATCH = 4
SEQ = 8192
DEPTH = 1

CHUNK = 64
Q_BLOCK = 128
HEAD_DIM = 64
N_ATTN_HEADS = 8
D_ATTN = N_ATTN_HEADS * HEAD_DIM
N_IDX_HEADS = 8
IDX_DIM = 64
TOPK_MAX = 256
D_CONV = D_MODEL - D_ATTN
N_CONV_GROUPS = 8
CONV_WIDTH = 3
D_FF = 2816
D_PLE = 256
LN_EPS = 1e-5
DN_ALPHA = (2 * DEPTH) ** 0.25
DN_BETA = (8 * DEPTH) ** -0.25
IDX_SCALE = (N_IDX_HEADS ** -0.5) * (IDX_DIM ** -0.5)
SPLIT_SIZES = (D_ATTN, D_ATTN, D_ATTN, N_IDX_HEADS * IDX_DIM, IDX_DIM, N_IDX_HEADS, D_CONV, D_CONV, D_CONV)
D_IN = sum(SPLIT_SIZES)

kernel_name = "hybrid_dsa_shortconv_macaron_deepnorm"


def alibi_slopes():
    h = jnp.arange(1, N_ATTN_HEADS + 1, dtype=jnp.float32)
    return jnp.exp2(-8.0 * h / N_ATTN_HEADS)


def layer_norm(x, g, b):
    xf = x.astype(jnp.float32)
    mu = jnp.mean(xf, axis=-1, keepdims=True)
    xc = xf - mu
    var = jnp.mean(xc * xc, axis=-1, keepdims=True)
    y = xc * lax.rsqrt(var + LN_EPS) * g.astype(jnp.float32) + b.astype(jnp.float32)
    return y.astype(x.dtype)


def swiglu(x, wg, wu, wd):
    return (jax.nn.silu(x @ wg) * (x @ wu)) @ wd


def short_conv_mixer(bg, cg, u, conv_w):
    s = u.shape[1]
    z = cg * u
    zp = jnp.pad(z, ((0, 0), (CONV_WIDTH - 1, 0), (0, 0)))
    y = sum(conv_w[j] * zp[:, j:j + s] for j in range(CONV_WIDTH))
    return bg * y


def dsa_attention(q, k, v, q_idx, k_idx, w_idx):
    b, s = q.shape[0], q.shape[1]
    topk = min(TOPK_MAX, s // 4)
    nb = s // Q_BLOCK
    slopes = alibi_slopes()
    key_pos = jnp.arange(s)
    k_idx_f = k_idx.astype(jnp.float32)

    def block(args):
        qb, qib, wb, start = args
        q_pos = start + jnp.arange(Q_BLOCK)
        limit = (q_pos // CHUNK + 1) * CHUNK
        rel = jnp.einsum('bqhd,bsd->bqhs', qib.astype(jnp.float32), k_idx_f)
        score = jnp.einsum('bqhs,bqh->bqs', jax.nn.relu(rel), wb.astype(jnp.float32))
        admissible = key_pos[None, :] < limit[:, None]
        score = jnp.where(admissible[None], score, -jnp.inf)
        _, idx = lax.top_k(score, topk)
        valid = idx < limit[None, :, None]
        kg = jax.vmap(lambda kb, ib: kb[ib])(k, idx)
        vg = jax.vmap(lambda vb, ib: vb[ib])(v, idx)
        logits = jnp.einsum('bqhd,bqkhd->bhqk', qb, kg).astype(jnp.float32) * (HEAD_DIM ** -0.5)
        dist = jnp.abs(q_pos[None, :, None] - idx).astype(jnp.float32)
        logits = logits - slopes[None, :, None, None] * dist[:, None]
        logits = jnp.where(valid[:, None], logits, -jnp.inf)
        probs = jax.nn.softmax(logits, axis=-1).astype(v.dtype)
        return jnp.einsum('bhqk,bqkhd->bqhd', probs, vg)

    def to_blocks(a):
        return a.reshape(b, nb, Q_BLOCK, *a.shape[2:]).swapaxes(0, 1)

    starts = jnp.arange(nb) * Q_BLOCK
    out = lax.map(block, (to_blocks(q), to_blocks(q_idx), to_blocks(w_idx), starts))
    return out.swapaxes(0, 1).reshape(b, s, N_ATTN_HEADS * HEAD_DIM)


def setup_inputs(seed: int = 0) -> dict:
    key = jax.random.key(seed)
    ks = jax.random.split(key, 24)
    f32 = jnp.float32

    def nrm(k, shape, scale):
        return jax.random.normal(k, shape, f32) * scale

    x = nrm(ks[0], (BATCH, SEQ, D_MODEL), 1.0)
    p = nrm(ks[1], (DEPTH, BATCH, SEQ, D_PLE), 1.0)
    w_in = nrm(ks[2], (DEPTH, D_MODEL, D_IN), D_MODEL ** -0.5)
    w_in = w_in.at[:, :, 2 * D_ATTN:3 * D_ATTN].multiply(DN_BETA)
    return {
        "x": x,
        "p": p,
        "ln1_g": 1.0 + nrm(ks[3], (DEPTH, D_MODEL), 0.02),
        "ln1_b": nrm(ks[4], (DEPTH, D_MODEL), 0.02),
        "ffn1_wg": nrm(ks[5], (DEPTH, D_MODEL, D_FF), D_MODEL ** -0.5),
        "ffn1_wu": nrm(ks[6], (DEPTH, D_MODEL, D_FF), DN_BETA * D_MODEL ** -0.5),
        "ffn1_wd": nrm(ks[7], (DEPTH, D_FF, D_MODEL), DN_BETA * D_FF ** -0.5),
        "w_in": w_in,
        "conv_w": nrm(ks[8], (DEPTH, CONV_WIDTH, D_CONV), CONV_WIDTH ** -0.5),
        "w_out": nrm(ks[9], (DEPTH, D_MODEL, D_MODEL), DN_BETA * D_MODEL ** -0.5),
        "ln2_g": 1.0 + nrm(ks[10], (DEPTH, D_MODEL), 0.02),
        "ln2_b": nrm(ks[11], (DEPTH, D_MODEL), 0.02),
        "ffn2_wg": nrm(ks[12], (DEPTH, D_MODEL, D_FF), D_MODEL ** -0.5),
        "ffn2_wu": nrm(ks[13], (DEPTH, D_MODEL, D_FF), DN_BETA * D_MODEL ** -0.5),
        "ffn2_wd": nrm(ks[14], (DEPTH, D_FF, D_MODEL), DN_BETA * D_FF ** -0.5),
        "ln3_g": 1.0 + nrm(ks[15], (DEPTH, D_MODEL), 0.02),
        "ln3_b": nrm(ks[16], (DEPTH, D_MODEL), 0.02),
        "ple_gate_w": nrm(ks[17], (DEPTH, D_MODEL, D_MODEL), D_MODEL ** -0.5),
        "ple_proj_w": nrm(ks[18], (DEPTH, D_PLE, D_MODEL), D_PLE ** -0.5),
    }


def reference(x, p, ln1_g, ln1_b, ffn1_wg, ffn1_wu, ffn1_wd, w_in, conv_w, w_out,
              ln2_g, ln2_b, ffn2_wg, ffn2_wu, ffn2_wd, ln3_g, ln3_b, ple_gate_w, ple_proj_w):
    b, s, _ = x.shape
    split_points = list(np.cumsum(SPLIT_SIZES)[:-1])
    for i in range(DEPTH):
        x = layer_norm(DN_ALPHA * x + 0.5 * swiglu(x, ffn1_wg[i], ffn1_wu[i], ffn1_wd[i]), ln1_g[i], ln1_b[i])
        h = x @ w_in[i]
        q, k, v, qi, ki, wi, bg, cg, u = jnp.split(h, split_points, axis=-1)
        attn = dsa_attention(
            q.reshape(b, s, N_ATTN_HEADS, HEAD_DIM),
            k.reshape(b, s, N_ATTN_HEADS, HEAD_DIM),
            v.reshape(b, s, N_ATTN_HEADS, HEAD_DIM),
            qi.reshape(b, s, N_IDX_HEADS, IDX_DIM),
            ki,
            wi * IDX_SCALE,
        )
        conv = short_conv_mixer(bg, cg, u, conv_w[i])
        mix = jnp.concatenate([attn, conv], axis=-1) @ w_out[i]
        x = layer_norm(DN_ALPHA * x + mix, ln2_g[i], ln2_b[i])
        x = layer_norm(DN_ALPHA * x + 0.5 * swiglu(x, ffn2_wg[i], ffn2_wu[i], ffn2_wd[i]), ln3_g[i], ln3_b[i])
        x = x + jax.nn.sigmoid(x @ ple_gate_w[i]) * (p[i] @ ple_proj_w[i])
    return x
```

```python
import numpy as np
from contextlib import ExitStack, contextmanager
import concourse.bass as bass
import concourse.mybir as mybir
from concourse.bass_utils import run_bass_kernel_spmd

F32 = mybir.dt.float32
BF16 = mybir.dt.bfloat16
ALU = mybir.AluOpType
AF = mybir.ActivationFunctionType
AX = mybir.AxisListType

D = 1024
KC = 8
DFF = 2816
FC = 22
DN_ALPHA = 2.0 ** 0.25
IDX_SCALE = (8 ** -0.5) * (64 ** -0.5)
LN_EPS = 1e-5
TOPK = 256
BIG = float(2 ** 20)
PSC = 2.0 ** -20
NEG = -1.0e30
NIT = 14
SLOPES = [2.0 ** (-(h + 1)) for h in range(8)]
CH_K, CH_KI, CH_Q, CH_QI, CH_BG, CH_CG, CH_U = 0, 4, 5, 9, 13, 17, 21
NCH = 25


class T:
    __slots__ = ("h", "name", "w", "r")

    def __init__(self, h, name=""):
        self.h = h
        self.name = name
        self.w = None
        self.r = []

    def __getitem__(self, k):
        return self.h[k]


class FW:
    NDMA = 24

    def __init__(self, nc, stack):
        self.nc = nc
        self.stack = stack
        self.eng = {"pe": nc.tensor, "act": nc.scalar, "dve": nc.vector, "pool": nc.gpsimd, "sp": nc.sync}
        self.sem = {}
        self.cnt = {}
        self.seen = {e: {} for e in self.eng}
        for e in self.eng:
            self.sem[e] = stack.enter_context(nc.semaphore("s_" + e))
            self.cnt[e] = 0
        self.dsem = [stack.enter_context(nc.semaphore("d%d" % i)) for i in range(self.NDMA)]
        self.dcnt = [0] * self.NDMA
        self.dnext = 0
        self.ninstr = 0
        self.uid = 0

    def sb(self, name, shape, dt, stack=None):
        st = stack or self.stack
        self.uid += 1
        return T(st.enter_context(self.nc.sbuf_tensor("%s_%d" % (name, self.uid), list(shape), dt)), name)

    def ps(self, name, shape, dt, stack=None):
        st = stack or self.stack
        self.uid += 1
        return T(st.enter_context(self.nc.psum_tensor("%s_%d" % (name, self.uid), list(shape), dt)), name)

    @contextmanager
    def scope(self):
        with ExitStack() as st:
            yield st
            self.barrier()

    def _wait(self, e, tok):
        if tok is None:
            return
        sem, val, src = tok
        key = id(sem)
        if self.seen[e].get(key, 0) >= val:
            return
        self.eng[e].wait_ge(sem, val)
        self.ninstr += 1
        self.seen[e][key] = val

    def _deps(self, e, reads, writes):
        for t in reads:
            self._wait(e, t.w)
        for t in writes:
            if not (e == "pe" and t.w is not None and t.w[2] == "pe"):
                self._wait(e, t.w)
            for r in t.r:
                if r[2] == e:
                    continue
                self._wait(e, r)

    def _record(self, tok, reads, writes):
        for t in reads:
            t.r.append(tok)
            if len(t.r) > 64:
                t.r = t.r[-48:]
        for t in writes:
            t.w = tok
            t.r = []

    def op(self, e, fn, reads=(), writes=()):
        self._deps(e, reads, writes)
        ins = fn(self.eng[e])
        self.cnt[e] += 1
        ins.then_inc(self.sem[e], 1)
        self.ninstr += 1
        tok = (self.sem[e], self.cnt[e], e)
        self._record(tok, reads, writes)
        return tok

    def mm(self, fns, reads=(), writes=()):
        e = "pe"
        self._deps(e, reads, writes)
        ins = None
        for fn in fns:
            ins = fn(self.eng[e])
            self.ninstr += 1
        self.cnt[e] += 1
        ins.then_inc(self.sem[e], 1)
        tok = (self.sem[e], self.cnt[e], e)
        self._record(tok, reads, writes)
        return tok

    def dma(self, q, out_ap, in_ap, reads=(), writes=()):
        i = self.dnext
        self.dnext = (self.dnext + 1) % self.NDMA
        if self.dcnt[i] > 0:
            self._wait(q, (self.dsem[i], self.dcnt[i], "dma"))
        for t in reads:
            self._wait(q, t.w)
        for t in writes:
            self._wait(q, t.w)
            for r in t.r:
                self._wait(q, r)
        self.dcnt[i] += 16
        self.eng[q].dma_start(out=out_ap, in_=in_ap).then_inc(self.dsem[i], 16)
        self.ninstr += 1
        tok = (self.dsem[i], self.dcnt[i], "dma")
        self._record(tok, reads, writes)
        return tok

    def barrier(self):
        toks = [(self.sem[e], self.cnt[e], e) for e in self.eng if self.cnt[e] > 0]
        toks += [(self.dsem[i], self.dcnt[i], "dma") for i in range(self.NDMA) if self.dcnt[i] > 0]
        for e in self.eng:
            for tok in toks:
                if tok[2] == e:
                    continue
                self._wait(e, tok)


def bc_mid(ap2d, n):
    N = ap2d.shape[-1]
    return ap2d.rearrange("p (o j) -> p o j", o=1).to_broadcast([128, n, N])


import os


def build_program(NT):
    NQB = NT * 4
    NH = NQB * 2
    NKT = NT * 2
    nc = bass.Bass("TRN2", target_bir_lowering=False)

    def din(name, shape, dt=F32):
        return nc.dram_tensor(name, list(shape), dt, kind="ExternalInput").ap()

    def dscr(name, shape, dt=BF16):
        return nc.dram_tensor(name, list(shape), dt, kind="Internal").ap()

    xT_own = din("xT_own", [NT, 128, 8, 512])
    xT_oth = din("xT_oth", [NT, 128, 8, 512])
    xT_halo = din("xT_halo", [128, 8, NH])
    pT_own = din("pT_own", [NT, 128, 2, 512])
    halo_valid = din("halo_valid", [128, NH])
    offrows_d = din("offrows", [128, 2, 512])
    tqk_d = din("tqk", [128, NQB * NT])
    limk_d = din("limk", [128, NQB])
    ident_d = din("ident", [128, 128])
    lnp_d = din("lnp", [128, 6 * 8])
    convw_d = din("convw", [128, 12])
    slopebig_d = din("slopebig", [128, 8])
    pow2_d = din("pow2", [128, NIT])
    wshapes = {
        "wg1": [FC, 128, 8, 128], "wu1": [FC, 128, 8, 128], "wd1": [8, 128, FC, 128],
        "wg2": [FC, 128, 8, 128], "wu2": [FC, 128, 8, 128], "wd2": [8, 128, FC, 128],
        "winst": [NCH, 128, 8, 128], "winv": [1, 128, 8, 512], "winwi": [1, 128, 8, 8],
        "wout": [8, 128, 8, 128], "wgate": [8, 128, 8, 128], "wproj": [8, 128, 2, 128],
    }
    w32 = {k: din(k, s) for k, s in wshapes.items()}
    wbf = {k: dscr(k + "_b", s) for k, s in wshapes.items()}
    wtok = {k: [T(None, "%s%d" % (k, i)) for i in range(s[0])] for k, s in wshapes.items()}
    KT_d = dscr("KT_d", [NKT, 128, 4, 512])
    V_d = dscr("V_d", [NKT, 128, 4, 512])
    X1_d = dscr("X1_d", [NT, 128, 8, 512], F32)
    Q_d = dscr("Q_d", [NT, 128, 4, 512])
    QI_d = dscr("QI_d", [NT, 128, 4, 512])
    WI_d = dscr("WI_d", [NT, 128, 32], F32)
    CONV_d = dscr("CONV_d", [NT, 128, 4, 512])
    ATT_d = dscr("ATT_d", [NT, 128, 4, 512])
    X1_tok = [T(None, "x1d%d" % i) for i in range(NT)]
    Q_tok = [T(None, "qd%d" % i) for i in range(NT)]
    QI_tok = [T(None, "qid%d" % i) for i in range(NT)]
    WI_tok = [T(None, "wid%d" % i) for i in range(NT)]
    CONV_tok = [T(None, "convd%d" % i) for i in range(NT)]
    ATT_tok = [T(None, "attd%d" % i) for i in range(NT)]
    KT_tok = [T(None, "ktd%d" % i) for i in range(NKT)]
    V_tok = [T(None, "vd%d" % i) for i in range(NKT)]
    outT = nc.dram_tensor("outT", [NT, 128, 8, 512], F32, kind="ExternalOutput").ap()
    out_tok = T(None, "out")

    with ExitStack() as st:
        fw = FW(nc, st)
        order = ["wg1", "wu1", "wd1", "winst", "winv", "winwi", "wout", "wg2", "wu2", "wd2", "wgate", "wproj"]
        for k in order:
            for i in range(wshapes[k][0]):
                fw.dma("pool", wbf[k][i], w32[k][i], writes=[wtok[k][i]])

        ki2 = fw.sb("ki2", [128, NKT * 512], BF16)
        ident32 = fw.sb("ident32", [128, 128], F32)
        identb = fw.sb("identb", [128, 128], BF16)
        ones32 = fw.sb("ones32", [128, 128], F32)
        onesb = fw.sb("onesb", [128, 128], BF16)
        lnp = fw.sb("lnp", [128, 48], F32)
        convw = fw.sb("convw", [128, 12], F32)
        slopebig = fw.sb("slopebig", [128, 8], F32)
        pow2 = fw.sb("pow2", [128, NIT], F32)
        offrows = fw.sb("offrows", [128, 2, 512], F32)
        tqk = fw.sb("tqk", [128, NQB * NT], F32)
        limk = fw.sb("limk", [128, NQB], F32)
        hval = fw.sb("hval", [128, NH], F32)
        zh = fw.sb("zh", [128, 4, NH], F32)
        bank = [fw.ps("bank%d" % i, [128, 512], F32) for i in range(7)]
        pbf_t = fw.ps("pbf", [128, 2, 512], BF16)
        pbfs = [T(pbf_t.h, "pbfA"), T(pbf_t.h, "pbfB")]
        PO = bank[6]

        for (t, d) in [(ident32, ident_d), (lnp, lnp_d), (convw, convw_d), (slopebig, slopebig_d), (pow2, pow2_d),
                       (offrows, offrows_d), (tqk, tqk_d), (limk, limk_d), (hval, halo_valid)]:
            fw.dma("sp", t[:], d, writes=[t])
        fw.op("dve", lambda e: e.tensor_copy(out=identb[:], in_=ident32[:]), reads=[ident32], writes=[identb])
        fw.op("dve", lambda e: e.memset(ones32[:], 1.0 / 1024.0), writes=[ones32])
        fw.op("dve", lambda e: e.memset(onesb[:], 1.0 / 1024.0), writes=[onesb])

        def alloc_ffn_scratch(stk, with_win):
            sc = {}
            sc["xb"] = fw.sb("xb", [128, 8, 512], BF16, stk)
            sc["hT"] = fw.sb("hT", [128, FC, 512], BF16, stk)
            sc["sg"] = [fw.sb("sg%d" % i, [128, 512], BF16, stk) for i in range(2)]
            sc["sq"] = fw.sb("sq", [128, 8, 512], BF16, stk)
            sc["mean"] = fw.sb("mean", [128, 512], F32, stk)
            sc["msq"] = fw.sb("msq", [128, 512], F32, stk)
            sc["rstd"] = fw.sb("rstd", [128, 512], F32, stk)
            sc["wgs"] = [fw.sb("wgs%d" % i, [128, 8, 128], BF16, stk) for i in range(2)]
            sc["wus"] = [fw.sb("wus%d" % i, [128, 8, 128], BF16, stk) for i in range(2)]
            sc["wds"] = [fw.sb("wds%d" % i, [128, FC, 128], BF16, stk) for i in range(2)]
            sc["wcs"] = [fw.sb("wcs%d" % i, [128, 8, 128], BF16, stk) for i in range(3)]
            sc["wci"] = 0
            sc["bki"] = 0
            sc["evi"] = 0
            if with_win:
                sc["wv_sb"] = fw.sb("wv_sb", [128, 8, 512], BF16, stk)
                sc["wwi_sb"] = fw.sb("wwi_sb", [128, 8, 8], BF16, stk)
                fw.dma("sp", sc["wv_sb"][:], wbf["winv"][0], reads=[wtok["winv"][0]], writes=[sc["wv_sb"]])
                fw.dma("sp", sc["wwi_sb"][:], wbf["winwi"][0], reads=[wtok["winwi"][0]], writes=[sc["wwi_sb"]])
                sc["cg_sb"] = fw.sb("cg_sb", [128, 512], F32, stk)
            return sc

        def load_chunk(sc, key, idx):
            s = sc["wcs"][sc["wci"] % 3]
            sc["wci"] += 1
            fw.dma("sp", s[:], wbf[key][idx], reads=[wtok[key][idx]], writes=[s])
            return s

        def nextbank(sc):
            b = bank[sc["bki"] % 6]
            sc["bki"] += 1
            return b

        def ffn(xt, N, kg, ku, kd, sc):
            hT, sg, xb = sc["hT"], sc["sg"], sc["xb"]
            wgs, wus, wds = sc["wgs"], sc["wus"], sc["wds"]

            def load_gu(c):
                fw.dma("sp", wgs[c % 2][:], wbf[kg][c], reads=[wtok[kg][c]], writes=[wgs[c % 2]])
                fw.dma("sp", wus[c % 2][:], wbf[ku][c], reads=[wtok[ku][c]], writes=[wus[c % 2]])

            def load_d(dc):
                fw.dma("sp", wds[dc % 2][:], wbf[kd][dc], reads=[wtok[kd][dc]], writes=[wds[dc % 2]])

            load_gu(0)
            for c in range(FC):
                if c + 1 < FC:
                    load_gu(c + 1)
                else:
                    load_d(0)
                pg, pu = bank[c % 2], bank[2 + c % 2]
                wg_, wu_ = wgs[c % 2], wus[c % 2]
                fw.mm([(lambda e, kc=kc: e.matmul(pg[:, :N], lhsT=wg_[:, kc, :], rhs=xb[:, kc, :N],
                                                  start=(kc == 0), stop=(kc == 7))) for kc in range(8)],
                      reads=[wg_, xb], writes=[pg])
                fw.mm([(lambda e, kc=kc: e.matmul(pu[:, :N], lhsT=wu_[:, kc, :], rhs=xb[:, kc, :N],
                                                  start=(kc == 0), stop=(kc == 7))) for kc in range(8)],
                      reads=[wu_, xb], writes=[pu])
                s_ = sg[c % 2]
                fw.op("act", lambda e: e.activation(out=s_[:, :N], in_=pg[:, :N], func=AF.Silu),
                      reads=[pg], writes=[s_])
                fw.op("dve", lambda e: e.tensor_tensor(out=hT[:, c, :N], in0=s_[:, :N], in1=pu[:, :N], op=ALU.mult),
                      reads=[s_, pu], writes=[hT])
            for dc in range(8):
                if dc + 1 < 8:
                    load_d(dc + 1)
                po = bank[4 + dc % 2]
                wd_ = wds[dc % 2]
                fw.mm([(lambda e, fc=fc: e.matmul(po[:, :N], lhsT=wd_[:, fc, :], rhs=hT[:, fc, :N],
                                                  start=(fc == 0), stop=(fc == FC - 1))) for fc in range(FC)],
                      reads=[wd_, hT], writes=[po])
                fw.op("dve", lambda e: e.scalar_tensor_tensor(out=xt[:, dc, :N], in0=xt[:, dc, :N],
                                                              scalar=2.0 * DN_ALPHA, in1=po[:, :N],
                                                              op0=ALU.mult, op1=ALU.add),
                      reads=[xt, po], writes=[xt])

        def layernorm(xt, N, li, eps, sc):
            sq, mean, msq, rstd, xb = sc["sq"], sc["mean"], sc["msq"], sc["rstd"], sc["xb"]
            fw.op("act", lambda e: e.activation(out=sq[:, :, :N], in_=xt[:, :, :N], func=AF.Square),
                  reads=[xt], writes=[sq])
            p1, p2 = bank[4], bank[5]
            fw.mm([(lambda e, kc=kc: e.matmul(p1[:, :N], lhsT=ones32[:], rhs=xt[:, kc, :N],
                                              start=(kc == 0), stop=(kc == 7))) for kc in range(8)],
                  reads=[ones32, xt], writes=[p1])
            fw.mm([(lambda e, kc=kc: e.matmul(p2[:, :N], lhsT=onesb[:], rhs=sq[:, kc, :N],
                                              start=(kc == 0), stop=(kc == 7))) for kc in range(8)],
                  reads=[onesb, sq], writes=[p2])
            fw.op("act", lambda e: e.copy(out=mean[:, :N], in_=p1[:, :N]), reads=[p1], writes=[mean])
            fw.op("dve", lambda e: e.tensor_tensor(out=msq[:, :N], in0=mean[:, :N], in1=mean[:, :N], op=ALU.mult),
                  reads=[mean], writes=[msq])
            fw.op("dve", lambda e: e.tensor_tensor(out=msq[:, :N], in0=p2[:, :N], in1=msq[:, :N], op=ALU.subtract),
                  reads=[p2, msq], writes=[msq])
            fw.op("dve", lambda e: e.tensor_scalar(out=msq[:, :N], in0=msq[:, :N], scalar1=eps, scalar2=None,
                                                   op0=ALU.add), reads=[msq], writes=[msq])
            fw.op("act", lambda e: e.activation(out=rstd[:, :N], in_=msq[:, :N], func=AF.Sqrt),
                  reads=[msq], writes=[rstd])
            fw.op("dve", lambda e: e.reciprocal(out=rstd[:, :N], in_=rstd[:, :N]), reads=[rstd], writes=[rstd])
            for hf in range(2):
                ks = slice(hf * 4, hf * 4 + 4)
                fw.op("dve", lambda e: e.tensor_tensor(out=xt[:, ks, :N], in0=xt[:, ks, :N], in1=bc_mid(mean[:, :N], 4),
                                                       op=ALU.subtract), reads=[xt, mean], writes=[xt])
                fw.op("dve", lambda e: e.tensor_tensor(out=xt[:, ks, :N], in0=xt[:, ks, :N], in1=bc_mid(rstd[:, :N], 4),
                                                       op=ALU.mult), reads=[xt, rstd], writes=[xt])
                for kc in range(hf * 4, hf * 4 + 4):
                    fw.op("act", lambda e, kc=kc: e.activation(out=xt[:, kc, :N], in_=xt[:, kc, :N], func=AF.Identity,
                                                                scale=lnp[:, (2 * li) * 8 + kc:(2 * li) * 8 + kc + 1],
                                                                bias=lnp[:, (2 * li + 1) * 8 + kc:(2 * li + 1) * 8 + kc + 1]),
                          reads=[xt, lnp], writes=[xt])
            fw.op("dve", lambda e: e.tensor_copy(out=xb[:, :, :N], in_=xt[:, :, :N]), reads=[xt], writes=[xb])

        def proj_st(sc, ci, N, n0=0):
            xb = sc["xb"]
            w_ = load_chunk(sc, "winst", ci)
            b = nextbank(sc)
            fw.mm([(lambda e, kc=kc: e.matmul(b[:, :N], lhsT=w_[:, kc, :], rhs=xb[:, kc, n0:n0 + N],
                                              start=(kc == 0), stop=(kc == 7))) for kc in range(8)],
                  reads=[w_, xb], writes=[b])
            return b

        def evac(sc, out_ap, in_ap, reads, writes, scale=None):
            sc["evi"] += 1
            if sc["evi"] % 2 == 0:
                if scale is None:
                    fw.op("act", lambda e: e.copy(out=out_ap, in_=in_ap), reads=reads, writes=writes)
                else:
                    fw.op("act", lambda e: e.mul(out=out_ap, in_=in_ap, mul=scale), reads=reads, writes=writes)
            else:
                if scale is None:
                    fw.op("dve", lambda e: e.tensor_copy(out=out_ap, in_=in_ap), reads=reads, writes=writes)
                else:
                    fw.op("dve", lambda e: e.tensor_scalar(out=out_ap, in0=in_ap, scalar1=scale, scalar2=None,
                                                           op0=ALU.mult), reads=reads, writes=writes)

        def conv_part(N, sc, own_T):
            cg_sb = sc["cg_sb"]
            for c in range(4):
                pcg = proj_st(sc, CH_CG + c, N)
                pu_ = proj_st(sc, CH_U + c, N)
                fw.op("act", lambda e: e.copy(out=cg_sb[:, :N], in_=pcg[:, :N]), reads=[pcg], writes=[cg_sb])
                if own_T is None:
                    fw.op("dve", lambda e: e.tensor_tensor(out=zh[:, c, :N], in0=cg_sb[:, :N], in1=pu_[:, :N],
                                                           op=ALU.mult), reads=[cg_sb, pu_], writes=[zh])
                    fw.op("dve", lambda e: e.tensor_tensor(out=zh[:, c, :N], in0=zh[:, c, :N], in1=hval[:, :N],
                                                           op=ALU.mult), reads=[zh, hval], writes=[zh])
                    continue
                z, acc = sc["z"], sc["acc"]
                pbg = proj_st(sc, CH_BG + c, N)
                fw.op("dve", lambda e: e.tensor_tensor(out=z[:, :, 2:130],
                                                       in0=cg_sb[:].rearrange("p (b j) -> p b j", b=4),
                                                       in1=pu_[:].rearrange("p (b j) -> p b j", b=4), op=ALU.mult),
                      reads=[cg_sb, pu_], writes=[z])
                fw.op("dve", lambda e: e.tensor_copy(
                    out=z[:, :, 0:2],
                    in_=zh[:, c, own_T * 8:(own_T + 1) * 8].rearrange("p (b k) -> p b k", k=2)),
                      reads=[zh, z], writes=[z])
                fw.op("dve", lambda e: e.tensor_scalar(out=acc[:], in0=z[:, :, 0:128],
                                                       scalar1=convw[:, c * 3 + 0:c * 3 + 1], scalar2=None,
                                                       op0=ALU.mult), reads=[z, convw], writes=[acc])
                for j in (1, 2):
                    fw.op("dve", lambda e, j=j: e.scalar_tensor_tensor(out=acc[:], in0=z[:, :, j:j + 128],
                                                                        scalar=convw[:, c * 3 + j:c * 3 + j + 1],
                                                                        in1=acc[:], op0=ALU.mult, op1=ALU.add),
                          reads=[z, convw, acc], writes=[acc])
                convT = sc["convT"]
                fw.op("dve", lambda e: e.tensor_tensor(out=convT[:, c, :].rearrange("p (b j) -> p b j", b=4),
                                                       in0=acc[:], in1=pbg[:].rearrange("p (b j) -> p b j", b=4),
                                                       op=ALU.mult), reads=[acc, pbg], writes=[convT])

        def kv_part(kt, sc):
            kt_sb, v_sb, xb, wv_sb = sc["kt_sb"], sc["v_sb"], sc["xb"], sc["wv_sb"]
            for j in range(4):
                b = proj_st(sc, CH_K + j, 512)
                evac(sc, kt_sb[:, j, :], b[:], [b], [kt_sb])
            fw.dma("sp", KT_d[kt], kt_sb[:], reads=[kt_sb], writes=[KT_tok[kt]])
            b = proj_st(sc, CH_KI, 512)
            evac(sc, ki2[:, kt * 512:(kt + 1) * 512], b[:], [b], [ki2])
            for blk in range(4):
                b = nextbank(sc)
                fw.mm([(lambda e, kc=kc: e.matmul(b[:], lhsT=xb[:, kc, blk * 128:(blk + 1) * 128], rhs=wv_sb[:, kc, :],
                                                  start=(kc == 0), stop=(kc == 7))) for kc in range(8)],
                      reads=[xb, wv_sb], writes=[b])
                evac(sc, v_sb[:, blk, :], b[:], [b], [v_sb])
            fw.dma("sp", V_d[kt], v_sb[:], reads=[v_sb], writes=[V_tok[kt]])

        def q_part(sc, Tt):
            xb, wwi_sb = sc["xb"], sc["wwi_sb"]
            qc, qic, wis = sc["qc"], sc["qic"], sc["wis"]
            for j in range(4):
                b = proj_st(sc, CH_Q + j, 512)
                evac(sc, qc[:, j, :], b[:], [b], [qc], scale=0.125)
            fw.dma("sp", Q_d[Tt], qc[:], reads=[qc], writes=[Q_tok[Tt]])
            for j in range(4):
                b = proj_st(sc, CH_QI + j, 512)
                evac(sc, qic[:, j, :], b[:], [b], [qic])
            fw.dma("sp", QI_d[Tt], qic[:], reads=[qic], writes=[QI_tok[Tt]])
            b = nextbank(sc)
            for blk in range(4):
                fw.mm([(lambda e, kc=kc: e.matmul(b[:, blk * 8:(blk + 1) * 8], lhsT=xb[:, kc, blk * 128:(blk + 1) * 128],
                                                  rhs=wwi_sb[:, kc, :], start=(kc == 0), stop=(kc == 7)))
                       for kc in range(8)], reads=[xb, wwi_sb], writes=[b])
            fw.op("dve", lambda e: e.tensor_scalar(out=wis[:], in0=b[:, 0:32], scalar1=IDX_SCALE, scalar2=None,
                                                   op0=ALU.mult), reads=[b], writes=[wis])
            fw.dma("sp", WI_d[Tt], wis[:], reads=[wis], writes=[WI_tok[Tt]])

        with fw.scope() as stk:
            sc = alloc_ffn_scratch(stk, True)
            xth = fw.sb("xth", [128, 8, 512], F32, stk)
            fw.dma("sp", xth[:, :, :NH], xT_halo, writes=[xth])
            fw.op("dve", lambda e: e.tensor_copy(out=sc["xb"][:, :, :NH], in_=xth[:, :, :NH]), reads=[xth], writes=[sc["xb"]])
            ffn(xth, NH, "wg1", "wu1", "wd1", sc)
            layernorm(xth, NH, 0, 4.0 * LN_EPS, sc)
            conv_part(NH, sc, None)

        with fw.scope() as stk:
            sc = alloc_ffn_scratch(stk, True)
            sc["z"] = fw.sb("z", [128, 4, 130], F32, stk)
            sc["acc"] = fw.sb("acc", [128, 4, 128], F32, stk)
            sc["kt_sb"] = fw.sb("kt_sb", [128, 4, 512], BF16, stk)
            sc["v_sb"] = fw.sb("v_sb", [128, 4, 512], BF16, stk)
            sc["qc"] = fw.sb("qc", [128, 4, 512], BF16, stk)
            sc["qic"] = fw.sb("qic", [128, 4, 512], BF16, stk)
            sc["wis"] = fw.sb("wis", [128, 32], F32, stk)
            sc["convT"] = fw.sb("convT", [128, 4, 512], BF16, stk)
            xts = [fw.sb("xts%d" % i, [128, 8, 512], F32, stk) for i in range(2)]
            xb = sc["xb"]
            for Tt in range(NT):
                for sub in (0, 1):
                    xt = xts[sub]
                    src = xT_own if sub == 0 else xT_oth
                    fw.dma("sp", xt[:], src[Tt], writes=[xt])
                    fw.op("dve", lambda e: e.tensor_copy(out=xb[:], in_=xt[:]), reads=[xt], writes=[xb])
                    ffn(xt, 512, "wg1", "wu1", "wd1", sc)
                    layernorm(xt, 512, 0, 4.0 * LN_EPS, sc)
                    if sub == 0:
                        fw.dma("sp", X1_d[Tt], xt[:], reads=[xt], writes=[X1_tok[Tt]])
                    kv_part(2 * Tt + sub, sc)
                    if sub == 0:
                        q_part(sc, Tt)
                        conv_part(512, sc, Tt)
                        fw.dma("sp", CONV_d[Tt], sc["convT"][:], reads=[sc["convT"]], writes=[CONV_tok[Tt]])

        with fw.scope() as stk:
            score = [fw.sb("score%d" % i, [128, NKT * 512], F32, stk) for i in range(2)]
            junk = fw.sb("junk", [128, max(3072, NKT * 256)], BF16, stk)
            junk2 = fw.sb("junk2", [128, NKT * 256], BF16, stk)
            sgn = [fw.sb("sgn%d" % i, [128, NIT], F32, stk) for i in range(2)]
            Rr = [fw.sb("R%d" % i, [128, 512], BF16, stk) for i in range(4)]
            Tts = [fw.sb("Tt%d" % i, [128, 512], F32, stk) for i in range(4)]
            Pp = [fw.sb("P%d" % i, [128, 512], BF16, stk) for i in range(4)]
            PTs = [fw.sb("PT%d" % i, [128, 4, 128], BF16, stk) for i in range(4)]
            SB4 = [bank[0], bank[1], bank[2], bank[5]]
            ktile = [fw.sb("ktile%d" % i, [128, 4, 512], BF16, stk) for i in range(2)]
            vtile = [fw.sb("vtile%d" % i, [128, 4, 512], BF16, stk) for i in range(2)]
            diag = fw.sb("diag", [128, 8, 128], BF16, stk)
            distn = [fw.sb("distn%d" % i, [128, 512], F32, stk) for i in range(2)]
            mb = fw.sb("mb", [128, 512], F32, stk)
            attn_sb = fw.sb("attn_sb", [128, 512], BF16, stk)
            amaxc = [fw.sb("amaxc%d" % i, [128, NKT], F32, stk) for i in range(2)]
            small = [fw.sb("small%d" % i, [128, 16], F32, stk) for i in range(2)]
            Wi = [fw.sb("Wi%d" % i, [128, NIT], F32, stk) for i in range(2)]
            cnt = [fw.sb("cnt%d" % i, [128, NIT], F32, stk) for i in range(2)]
            rowsum = fw.sb("rowsum", [128, 8 * NKT], F32, stk)
            rs = fw.sb("rs", [128, 8], F32, stk)
            biasc = fw.sb("biasc", [128, 8], F32, stk)
            qXs = [[fw.sb("qX%d_%d" % (s_, i), [128, 4, 512], BF16, stk) for i in range(2)] for s_ in range(2)]
            qiXs = [[fw.sb("qiX%d_%d" % (s_, i), [128, 4, 512], BF16, stk) for i in range(2)] for s_ in range(2)]
            wi_sbs = [fw.sb("wi_sb%d" % s_, [128, 32], F32, stk) for s_ in range(2)]
            attT = [fw.sb("attT%d" % s_, [128, 4, 512], BF16, stk) for s_ in range(2)]
            for s_ in range(2):
                for t in qXs[s_] + qiXs[s_]:
                    fw.op("dve", lambda e, t=t: e.memset(t[:], 0.0), writes=[t])

            def load_tile_q(Tt):
                s_ = Tt % 2
                for eh in range(2):
                    ps = slice(eh * 64, (eh + 1) * 64)
                    fw.dma("sp", qXs[s_][eh][ps], Q_d[Tt][ps], reads=[Q_tok[Tt]], writes=[qXs[s_][eh]])
                    fw.dma("sp", qiXs[s_][eh][ps], QI_d[Tt][ps], reads=[QI_tok[Tt]], writes=[qiXs[s_][eh]])
                fw.dma("sp", wi_sbs[s_][:], WI_d[Tt], reads=[WI_tok[Tt]], writes=[wi_sbs[s_]])

            def gen_indexer(g):
                Tt, qb = divmod(g, 4)
                nkt = 2 * Tt + 2
                if qb == 0:
                    load_tile_q(Tt)
                par = g % 2
                qiX, wi_sb = qiXs[Tt % 2], wi_sbs[Tt % 2]
                qs = slice(qb * 128, (qb + 1) * 128)
                sco, amx = score[par], amaxc[par]
                for h in range(8):
                    fw.op("dve", lambda e, h=h: e.tensor_scalar(out=diag[:, h, :], in0=identb[:],
                                                                 scalar1=wi_sb[:, qb * 8 + h:qb * 8 + h + 1],
                                                                 scalar2=None, op0=ALU.mult),
                          reads=[identb, wi_sb], writes=[diag])
                items = [(kt, h) for kt in range(nkt) for h in range(8)]
                n = len(items)

                def A(i):
                    kt, h = items[i]
                    hp, eh = divmod(h, 2)
                    pr = SB4[i % 4]
                    fw.mm([lambda e: e.matmul(pr[:], lhsT=qiX[eh][:, hp, qs], rhs=ki2[:, kt * 512:(kt + 1) * 512],
                                              start=True, stop=True)], reads=[qiX[eh], ki2], writes=[pr])

                def B(i):
                    pr, r_ = SB4[i % 4], Rr[i % 4]
                    fw.op("act", lambda e: e.activation(out=r_[:], in_=pr[:], func=AF.Relu), reads=[pr], writes=[r_])

                def C(i):
                    kt, h = items[i]
                    psc, r_ = bank[3 + kt % 2], Rr[i % 4]
                    fw.mm([lambda e: e.matmul(psc[:], lhsT=diag[:, h, :], rhs=r_[:], start=(h == 0), stop=(h == 7))],
                          reads=[diag, r_], writes=[psc])
                    if h == 7:
                        sl = slice(kt * 512, (kt + 1) * 512)
                        fw.op("dve", lambda e: e.tensor_copy(out=sco[:, sl], in_=psc[:]), reads=[psc], writes=[sco])
                        fw.op("dve", lambda e: e.tensor_reduce(out=amx[:, kt:kt + 1], in_=sco[:, sl], axis=AX.X,
                                                               op=ALU.max, apply_absolute_value=True),
                              reads=[sco], writes=[amx])
                        if kt >= 2 * Tt:
                            sub = kt - 2 * Tt
                            fw.op("dve", lambda e: e.tensor_scalar(out=mb[:], in0=offrows[:, sub, :],
                                                                   scalar1=limk[:, g:g + 1], scalar2=NEG,
                                                                   op0=ALU.is_ge, op1=ALU.mult),
                                  reads=[offrows, limk], writes=[mb])
                            fw.op("dve", lambda e: e.tensor_tensor(out=sco[:, sl], in0=sco[:, sl], in1=mb[:],
                                                                   op=ALU.add), reads=[sco, mb], writes=[sco])
                for j in range(min(3, n)):
                    A(j)
                for i in range(n):
                    if i + 3 < n:
                        A(i + 3)
                    B(i)
                    C(i)
                    if items[i][1] == 7:
                        yield

            def gen_bisect(g):
                Tt, qb = divmod(g, 4)
                nkt = 2 * Tt + 2
                L = nkt * 512
                par = g % 2
                sco, amx, sm, wi_, cn, sn = score[par], amaxc[par], small[par], Wi[par], cnt[par], sgn[par]
                L1 = L // 2 if L >= 3072 else L
                fw.op("dve", lambda e: e.memset(cn[:], 0.0), writes=[cn])
                fw.op("dve", lambda e: e.memset(sn[:], 0.0), writes=[sn])
                fw.op("dve", lambda e: e.tensor_reduce(out=sm[:, 0:1], in_=amx[:, :nkt], axis=AX.X, op=ALU.max),
                      reads=[amx], writes=[sm])
                fw.op("dve", lambda e: e.tensor_scalar(out=sm[:, 1:2], in0=sm[:, 0:1], scalar1=2.02,
                                                       scalar2=2e-6, op0=ALU.mult, op1=ALU.add),
                      reads=[sm], writes=[sm])
                fw.op("dve", lambda e: e.tensor_scalar(out=wi_[:], in0=pow2[:], scalar1=sm[:, 1:2], scalar2=None,
                                                       op0=ALU.mult), reads=[pow2, sm], writes=[wi_])
                fw.op("dve", lambda e: e.memset(sm[:, 2:3], 0.0), reads=[sm], writes=[sm])
                for it in range(NIT):
                    if L1 < L:
                        fw.op("act", lambda e: e.activation(out=junk2[:, :L - L1], in_=sco[:, L1:L], func=AF.Sign, scale=-1.0,
                                                            bias=sm[:, 2:3], accum_out=sn[:, it:it + 1]),
                              reads=[sco, sm, sn], writes=[junk2])
                    fw.op("dve", lambda e: e.tensor_scalar(out=junk[:, :L1], in0=sco[:, :L1],
                                                           scalar1=sm[:, 2:3], scalar2=0.0,
                                                           op0=ALU.is_ge, op1=ALU.add,
                                                           accum_out=cn[:, it:it + 1]),
                          reads=[sco, sm, cn], writes=[junk, cn])
                    fw.op("dve", lambda e: e.scalar_tensor_tensor(out=sm[:, 6:7], in0=sn[:, it:it + 1], scalar=-0.5,
                                                                  in1=cn[:, it:it + 1], op0=ALU.mult, op1=ALU.add),
                          reads=[sn, cn, sm, junk2], writes=[sm])
                    fw.op("dve", lambda e: e.tensor_scalar(out=sm[:, 3:4], in0=sm[:, 6:7],
                                                           scalar1=float(TOPK) - 0.5 * (L - L1), scalar2=-0.5,
                                                           op0=ALU.is_ge, op1=ALU.add),
                          reads=[sm], writes=[sm])
                    fw.op("dve", lambda e: e.scalar_tensor_tensor(out=sm[:, 2:3], in0=sm[:, 3:4],
                                                                  scalar=wi_[:, it:it + 1], in1=sm[:, 2:3],
                                                                  op0=ALU.mult, op1=ALU.add),
                          reads=[sm, wi_], writes=[sm])
                    yield
                fw.op("dve", lambda e: e.scalar_tensor_tensor(out=sm[:, 4:5], in0=wi_[:, NIT - 1:NIT], scalar=-0.5,
                                                              in1=sm[:, 2:3], op0=ALU.mult, op1=ALU.add),
                      reads=[sm, wi_], writes=[sm])

            def gen_estep(g):
                Tt, qb = divmod(g, 4)
                nkt = 2 * Tt + 2
                L = nkt * 512
                par = g % 2
                sco, sm = score[par], small[par]
                for kt in range(nkt):
                    Tk, sub = divmod(kt, 2)
                    dn = distn[kt % 2]
                    sl = slice(kt * 512, (kt + 1) * 512)
                    fw.op("act", lambda e: e.activation(out=dn[:], in_=offrows[:, sub, :], func=AF.Abs,
                                                        bias=tqk[:, g * NT + Tk:g * NT + Tk + 1], scale=1.0),
                          reads=[offrows, tqk], writes=[dn])
                    fw.op("dve", lambda e: e.scalar_tensor_tensor(out=sco[:, sl], in0=sco[:, sl],
                                                                  scalar=sm[:, 4:5], in1=dn[:],
                                                                  op0=ALU.is_lt, op1=ALU.add),
                          reads=[sco, sm, dn], writes=[sco])
                    yield
                fw.op("dve", lambda e: e.tensor_reduce(out=sm[:, 5:6], in_=sco[:, :L], axis=AX.X, op=ALU.min),
                      reads=[sco], writes=[sm])
                fw.op("dve", lambda e: e.tensor_scalar(out=biasc[:], in0=slopebig[:], scalar1=sm[:, 5:6],
                                                       scalar2=None, op0=ALU.mult),
                      reads=[slopebig, sm], writes=[biasc])

            def gen_attn(g):
                Tt, qb = divmod(g, 4)
                nkt = 2 * Tt + 2
                par = g % 2
                qX = qXs[Tt % 2]
                at_ = attT[Tt % 2]
                qs = slice(qb * 128, (qb + 1) * 128)
                sco = score[par]
                fw.op("dve", lambda e: e.memset(rowsum[:], 0.0), writes=[rowsum])
                items = [(kt, h) for kt in range(nkt) for h in range(8)]
                n = len(items)

                def load_kv(kt):
                    fw.dma("sp", ktile[kt % 2][:], KT_d[kt], reads=[KT_tok[kt]], writes=[ktile[kt % 2]])
                    fw.dma("sp", vtile[kt % 2][:], V_d[kt], reads=[V_tok[kt]], writes=[vtile[kt % 2]])

                def A(i):
                    kt, h = items[i]
                    hp, eh = divmod(h, 2)
                    ps_, kt_ = SB4[i % 4], ktile[kt % 2]
                    fw.mm([lambda e: e.matmul(ps_[:], lhsT=qX[eh][:, hp, qs], rhs=kt_[:, hp, :], start=True, stop=True)],
                          reads=[qX[eh], kt_], writes=[ps_])

                def BCDE(i):
                    kt, h = items[i]
                    sl = slice(kt * 512, (kt + 1) * 512)
                    ps_, t_, p_, pt_ = SB4[i % 4], Tts[i % 4], Pp[i % 4], PTs[i % 4]
                    hb = i % 2
                    pbf = pbfs[hb]
                    fw.op("dve", lambda e: e.scalar_tensor_tensor(out=t_[:], in0=sco[:, sl], scalar=-SLOPES[h] * BIG,
                                                                  in1=ps_[:], op0=ALU.mult, op1=ALU.add),
                          reads=[sco, ps_], writes=[t_])
                    fw.op("act", lambda e: e.activation(out=p_[:], in_=t_[:], func=AF.Exp, bias=biasc[:, h:h + 1], scale=1.0,
                                                        accum_out=rowsum[:, h * NKT + kt:h * NKT + kt + 1]),
                          reads=[t_, biasc, rowsum], writes=[p_])
                    fw.mm([(lambda e, sbk=sbk: e.transpose(out=pbf[:, hb, sbk * 128:(sbk + 1) * 128],
                                                           in_=p_[:, sbk * 128:(sbk + 1) * 128], identity=identb[:]))
                           for sbk in range(4)], reads=[p_, identb], writes=[pbf])
                    src_ap = pbf[:, hb, :].rearrange("p (s q) -> p s q", s=4)
                    if i % 2 == 0:
                        fw.op("act", lambda e: e.copy(out=pt_[:], in_=src_ap), reads=[pbf], writes=[pt_])
                    else:
                        fw.op("dve", lambda e: e.tensor_copy(out=pt_[:], in_=src_ap), reads=[pbf], writes=[pt_])

                def Fm(i):
                    kt, h = items[i]
                    pt_, vt_ = PTs[i % 4], vtile[kt % 2]
                    fw.mm([(lambda e, sbk=sbk: e.matmul(PO[:, h * 64:(h + 1) * 64], lhsT=pt_[:, sbk, :],
                                                        rhs=vt_[:, sbk, h * 64:(h + 1) * 64],
                                                        start=(i == 0 and sbk == 0), stop=(i == n - 1 and sbk == 3)))
                           for sbk in range(4)], reads=[pt_, vt_], writes=[PO])
                load_kv(0)
                if nkt > 1:
                    load_kv(1)
                for j in range(min(3, n)):
                    A(j)
                for i in range(n):
                    if i + 3 < n:
                        A(i + 3)
                    BCDE(i)
                    if i >= 1:
                        Fm(i - 1)
                        kt_prev, h_prev = items[i - 1]
                        if h_prev == 7 and kt_prev + 2 < nkt:
                            load_kv(kt_prev + 2)
                    yield
                Fm(n - 1)
                fw.op("dve", lambda e: e.tensor_reduce(out=rs[:], in_=rowsum[:].rearrange("p (h k) -> p h k", k=NKT),
                                                       axis=AX.X, op=ALU.add), reads=[rowsum], writes=[rs, rowsum])
                fw.op("dve", lambda e: e.reciprocal(out=rs[:], in_=rs[:]), reads=[rs], writes=[rs])
                fw.op("dve", lambda e: e.tensor_tensor(
                    out=attn_sb[:].rearrange("p (h d) -> p h d", d=64),
                    in0=PO[:].rearrange("p (h d) -> p h d", d=64),
                    in1=rs[:].rearrange("p (h o) -> p h o", o=1).to_broadcast([128, 8, 64]), op=ALU.mult),
                      reads=[PO, rs], writes=[attn_sb])
                pbf = pbfs[0]
                fw.mm([(lambda e, c=c: e.transpose(out=pbf[:, 0, c * 128:(c + 1) * 128],
                                                   in_=attn_sb[:, c * 128:(c + 1) * 128], identity=identb[:]))
                       for c in range(4)], reads=[attn_sb, identb], writes=[pbf])
                fw.op("act", lambda e: e.copy(out=at_[:, :, qs],
                                              in_=pbf[:, 0, :].rearrange("p (c q) -> p c q", c=4)),
                      reads=[pbf], writes=[at_])
                if qb == 3:
                    fw.dma("sp", ATT_d[Tt], at_[:], reads=[at_], writes=[ATT_tok[Tt]])

            def run_pair(ga, na, gb, nb):
                a_alive, b_alive = ga is not None, gb is not None
                ia = ib = 0
                while a_alive or b_alive:
                    pa = (ia / na) if a_alive else 2.0
                    pb = (ib / nb) if b_alive else 2.0
                    if pa <= pb:
                        try:
                            next(ga)
                            ia += 1
                        except StopIteration:
                            a_alive = False
                    else:
                        try:
                            next(gb)
                            ib += 1
                        except StopIteration:
                            b_alive = False

            def nkt_of(g):
                return 2 * (g // 4) + 2

            for _ in gen_indexer(0):
                pass
            for g in range(NQB):
                run_pair(gen_bisect(g), NIT, gen_attn(g - 1) if g >= 1 else None, 8 * nkt_of(max(g - 1, 0)))
                run_pair(gen_indexer(g + 1) if g + 1 < NQB else None, nkt_of(min(g + 1, NQB - 1)),
                         gen_estep(g), nkt_of(g))
            for _ in gen_attn(NQB - 1):
                pass

        with fw.scope() as stk:
            sc = alloc_ffn_scratch(stk, False)
            xb = sc["xb"]
            x1s = [fw.sb("x1s%d" % i, [128, 8, 512], F32, stk) for i in range(2)]
            cats = [fw.sb("cats%d" % i, [128, 8, 512], BF16, stk) for i in range(2)]
            pt32s = [fw.sb("pt32_%d" % i, [128, 2, 512], F32, stk) for i in range(2)]
            pb = fw.sb("pb", [128, 2, 512], BF16, stk)
            sgm = [fw.sb("sgm%d" % i, [128, 512], F32, stk) for i in range(2)]
            wps = [fw.sb("wps%d" % i, [128, 2, 128], BF16, stk) for i in range(2)]

            def p3_loads(Tt):
                s_ = Tt % 2
                fw.dma("sp", x1s[s_][:], X1_d[Tt], reads=[X1_tok[Tt]], writes=[x1s[s_]])
                fw.dma("sp", cats[s_][:, 0:4, :], ATT_d[Tt], reads=[ATT_tok[Tt]], writes=[cats[s_]])
                fw.dma("sp", cats[s_][:, 4:8, :], CONV_d[Tt], reads=[CONV_tok[Tt]], writes=[cats[s_]])
                fw.dma("sp", pt32s[s_][:], pT_own[Tt], writes=[pt32s[s_]])
            p3_loads(0)
            for Tt in range(NT):
                if Tt + 1 < NT:
                    p3_loads(Tt + 1)
                x1o, catT, pt32 = x1s[Tt % 2], cats[Tt % 2], pt32s[Tt % 2]
                fw.op("dve", lambda e: e.tensor_copy(out=pb[:], in_=pt32[:]), reads=[pt32], writes=[pb])
                for dc in range(8):
                    w_ = load_chunk(sc, "wout", dc)
                    b = nextbank(sc)
                    fw.mm([(lambda e, fc=fc: e.matmul(b[:], lhsT=w_[:, fc, :], rhs=catT[:, fc, :],
                                                      start=(fc == 0), stop=(fc == 7))) for fc in range(8)],
                          reads=[w_, catT], writes=[b])
                    fw.op("dve", lambda e: e.scalar_tensor_tensor(out=x1o[:, dc, :], in0=x1o[:, dc, :], scalar=DN_ALPHA,
                                                                  in1=b[:], op0=ALU.mult, op1=ALU.add),
                          reads=[x1o, b], writes=[x1o])
                layernorm(x1o, 512, 1, LN_EPS, sc)
                ffn(x1o, 512, "wg2", "wu2", "wd2", sc)
                layernorm(x1o, 512, 2, 4.0 * LN_EPS, sc)
                for dc in range(8):
                    w_ = load_chunk(sc, "wgate", dc)
                    wp_ = wps[dc % 2]
                    sg_ = sgm[dc % 2]
                    fw.dma("sp", wp_[:], wbf["wproj"][dc], reads=[wtok["wproj"][dc]], writes=[wp_])
                    bg_ = nextbank(sc)
                    bp_ = nextbank(sc)
                    fw.mm([(lambda e, fc=fc: e.matmul(bg_[:], lhsT=w_[:, fc, :], rhs=xb[:, fc, :],
                                                      start=(fc == 0), stop=(fc == 7))) for fc in range(8)],
                          reads=[w_, xb], writes=[bg_])
                    fw.mm([(lambda e, fc=fc: e.matmul(bp_[:], lhsT=wp_[:, fc, :], rhs=pb[:, fc, :],
                                                      start=(fc == 0), stop=(fc == 1))) for fc in range(2)],
                          reads=[wp_, pb], writes=[bp_])
                    fw.op("act", lambda e: e.activation(out=sg_[:], in_=bg_[:], func=AF.Sigmoid), reads=[bg_], writes=[sg_])
                    fw.op("dve", lambda e: e.tensor_tensor(out=sg_[:], in0=sg_[:], in1=bp_[:], op=ALU.mult),
                          reads=[sg_, bp_], writes=[sg_])
                    fw.op("dve", lambda e: e.tensor_tensor(out=x1o[:, dc, :], in0=x1o[:, dc, :], in1=sg_[:], op=ALU.add),
                          reads=[x1o, sg_], writes=[x1o])
                fw.dma("sp", outT[Tt], x1o[:], reads=[x1o], writes=[out_tok])
        fw.barrier()
    return nc, fw


def _tile_w(W, kc, cc):
    return np.ascontiguousarray(W.reshape(kc, 128, cc, 128).transpose(2, 1, 0, 3))


def prep_weights(inp):
    f = np.float32
    w = {}
    w["wg1"] = _tile_w(inp["ffn1_wg"][0], 8, FC)
    w["wu1"] = _tile_w(inp["ffn1_wu"][0], 8, FC)
    w["wd1"] = _tile_w(inp["ffn1_wd"][0], FC, 8)
    w["wg2"] = _tile_w(inp["ffn2_wg"][0], 8, FC)
    w["wu2"] = _tile_w(inp["ffn2_wu"][0], 8, FC)
    w["wd2"] = _tile_w(inp["ffn2_wd"][0], FC, 8)
    win = inp["w_in"][0]
    cols = {"q": (0, 512), "k": (512, 1024), "v": (1024, 1536), "qi": (1536, 2048), "ki": (2048, 2112),
            "wi": (2112, 2120), "bg": (2120, 2632), "cg": (2632, 3144), "u": (3144, 3656)}

    def sub(n):
        a, b = cols[n]
        return win[:, a:b]
    st = np.concatenate([sub("k"), sub("ki"), sub("ki"), sub("q"), sub("qi"), sub("bg"), sub("cg"), sub("u")], axis=1)
    assert st.shape[1] == NCH * 128
    w["winst"] = _tile_w(st, 8, NCH)
    w["winv"] = np.ascontiguousarray(sub("v").reshape(8, 128, 512).transpose(1, 0, 2))[None]
    w["winwi"] = np.ascontiguousarray(sub("wi").reshape(8, 128, 8).transpose(1, 0, 2))[None]
    w["wout"] = _tile_w(inp["w_out"][0], 8, 8)
    w["wgate"] = _tile_w(inp["ple_gate_w"][0], 8, 8)
    w["wproj"] = _tile_w(inp["ple_proj_w"][0], 2, 8)
    lnp = np.stack([inp["ln1_g"][0], inp["ln1_b"][0], inp["ln2_g"][0], inp["ln2_b"][0],
                    inp["ln3_g"][0], inp["ln3_b"][0]], 0)
    w["lnp"] = np.ascontiguousarray(lnp.reshape(6, 8, 128).transpose(2, 0, 1).reshape(128, 48))
    cw = inp["conv_w"][0]
    w["convw"] = np.ascontiguousarray(cw.reshape(3, 4, 128).transpose(2, 1, 0).reshape(128, 12))
    w["ident"] = np.eye(128, dtype=f)
    w["slopebig"] = np.tile(np.array([s * BIG for s in SLOPES], dtype=f)[None, :], (128, 1))
    w["pow2"] = np.tile(np.array([2.0 ** (-(i + 1)) for i in range(NIT)], dtype=f)[None, :], (128, 1))
    return {k: np.ascontiguousarray(v, dtype=f) for k, v in w.items()}


def core_blocks(NT, role):
    own, oth = [], []
    for t in range(NT):
        b = 8 * t
        a = [b, b + 3, b + 4, b + 7]
        o = [b + 1, b + 2, b + 5, b + 6]
        if role == 0:
            own.append(a); oth.append(o)
        else:
            own.append(o); oth.append(a)
    return own, oth


def prep_core(x_b, p_b, NT, role):
    f = np.float32
    own, oth = core_blocks(NT, role)
    ar = np.arange(128)

    def pos_of(blocks):
        return np.concatenate([b * 128 + ar for b in blocks])
    own_pos = [pos_of(b) for b in own]
    oth_pos = [pos_of(b) for b in oth]

    def featmajor(rows, kc):
        return np.ascontiguousarray(rows.T.reshape(kc, 128, rows.shape[0]).transpose(1, 0, 2))
    m = {}
    m["xT_own"] = np.stack([featmajor(x_b[p], 8) for p in own_pos], 0)
    m["xT_oth"] = np.stack([featmajor(x_b[p], 8) for p in oth_pos], 0)
    m["pT_own"] = np.stack([featmajor(p_b[p], 2) for p in own_pos], 0)
    hpos, hval = [], []
    for t in range(NT):
        for b in own[t]:
            s = b * 128
            if s == 0:
                hpos += [0, 1]; hval += [0.0, 0.0]
            else:
                hpos += [s - 2, s - 1]; hval += [1.0, 1.0]
    m["xT_halo"] = featmajor(x_b[np.array(hpos)], 8)
    m["halo_valid"] = np.tile(np.array(hval, dtype=f)[None, :], (128, 1))
    off = np.stack([own_pos[0], oth_pos[0]], 0).astype(np.float64)
    m["offrows"] = np.tile((off * PSC)[None], (128, 1, 1))
    NQB = NT * 4
    tqk = np.zeros((128, NQB, NT), dtype=np.float64)
    limk = np.zeros((128, NQB), dtype=np.float64)
    for t in range(NT):
        for qb in range(4):
            pq = own_pos[t][qb * 128:(qb + 1) * 128].astype(np.float64)
            for tk in range(NT):
                tqk[:, t * 4 + qb, tk] = -(pq - 1024.0 * tk) * PSC
            lim = (np.floor(pq / 64) + 1) * 64
            limk[:, t * 4 + qb] = (lim - 1024.0 * t) * PSC
    m["tqk"] = tqk.reshape(128, NQB * NT)
    m["limk"] = limk
    return {k: np.ascontiguousarray(v, dtype=f) for k, v in m.items()}, own_pos


_CACHE = {}


def run_module(inputs, NT, B):
    x = np.asarray(inputs["x"], dtype=np.float32)
    p = np.asarray(inputs["p"], dtype=np.float32)[0]
    wts = prep_weights({k: np.asarray(v, dtype=np.float32) for k, v in inputs.items() if k not in ("x", "p")})
    in_maps, poss = [], []
    for b in range(B):
        for role in (0, 1):
            m, own_pos = prep_core(x[b], p[b], NT, role)
            m.update(wts)
            in_maps.append(m)
            poss.append((b, own_pos))
    if NT not in _CACHE:
        _CACHE[NT] = build_program(NT)
    nc, fw = _CACHE[NT]
    import time as _t
    _t0 = _t.time()
    res = run_bass_kernel_spmd(nc, in_maps, core_ids=list(range(2 * B)), trace=bool(os.environ.get('KTRACE')))
    if os.environ.get('KTRACE'):
        print('exec_time_ns', res.exec_time_ns, flush=True)
    if os.environ.get('KVERB'):
        print("ninstr", fw.ninstr, "spmd run s", _t.time() - _t0, flush=True)
    if os.environ.get('KRAW'):
        return [np.asarray(r["outT"]) for r in res.results]
    out = np.zeros((B, NT * 1024, D), dtype=np.float32)
    for ci, (b, own_pos) in enumerate(poss):
        oT = np.asarray(res.results[ci]["outT"])
        for t in range(NT):
            rows = oT[t].transpose(2, 1, 0).reshape(512, D)
            out[b, own_pos[t]] = rows
    return out


def kernel(**inputs):
    return run_module(inputs, 8, 4)
```

```python
import numpy as np
from contextlib import ExitStack, contextmanager
import concourse.bass as bass
import concourse.mybir as mybir
from concourse.bass_utils import run_bass_kernel_spmd

F32 = mybir.dt.float32
BF16 = mybir.dt.bfloat16
ALU = mybir.AluOpType
AF = mybir.ActivationFunctionType
AX = mybir.AxisListType

D = 1024
KC = 8
DFF = 2816
FC = 22
DN_ALPHA = 2.0 ** 0.25
IDX_SCALE = (8 ** -0.5) * (64 ** -0.5)
LN_EPS = 1e-5
TOPK = 256
BIG = float(2 ** 20)
PSC = 2.0 ** -20
NEG = -1.0e30
NIT = 14
SLOPES = [2.0 ** (-(h + 1)) for h in range(8)]
CH_K, CH_KI, CH_Q, CH_QI, CH_BG, CH_CG, CH_U = 0, 4, 5, 9, 13, 17, 21
NCH = 25


class T:
    __slots__ = ("h", "name", "w", "r")

    def __init__(self, h, name=""):
        self.h = h
        self.name = name
        self.w = None
        self.r = []

    def __getitem__(self, k):
        return self.h[k]


class FW:
    NDMA = 32
    NSW = 8

    def __init__(self, nc, stack):
        self.nc = nc
        self.stack = stack
        self.eng = {"pe": nc.tensor, "act": nc.scalar, "dve": nc.vector, "pool": nc.gpsimd, "sp": nc.sync}
        self.sem = {}
        self.cnt = {}
        self.seen = {e: {} for e in self.eng}
        for e in self.eng:
            self.sem[e] = stack.enter_context(nc.semaphore("s_" + e))
            self.cnt[e] = 0
        self.dsem = [stack.enter_context(nc.semaphore("d%d" % i)) for i in range(self.NDMA)]
        self.dcnt = [0] * self.NDMA
        self.dnext = 0
        self.dnext_sw = 0
        self.ninstr = 0
        self.uid = 0

    def sb(self, name, shape, dt, stack=None):
        st = stack or self.stack
        self.uid += 1
        return T(st.enter_context(self.nc.sbuf_tensor("%s_%d" % (name, self.uid), list(shape), dt)), name)

    def ps(self, name, shape, dt, stack=None):
        st = stack or self.stack
        self.uid += 1
        return T(st.enter_context(self.nc.psum_tensor("%s_%d" % (name, self.uid), list(shape), dt)), name)

    @contextmanager
    def scope(self):
        with ExitStack() as st:
            yield st
            self.barrier()

    def _wait(self, e, tok):
        if tok is None:
            return
        sem, val, src = tok
        key = id(sem)
        if self.seen[e].get(key, 0) >= val:
            return
        self.eng[e].wait_ge(sem, val)
        self.ninstr += 1
        self.seen[e][key] = val

    def _deps(self, e, reads, writes):
        for t in reads:
            self._wait(e, t.w)
        for t in writes:
            if not (e == "pe" and t.w is not None and t.w[2] == "pe"):
                self._wait(e, t.w)
            for r in t.r:
                if r[2] == e:
                    continue
                self._wait(e, r)

    def _record(self, tok, reads, writes):
        for t in reads:
            t.r.append(tok)
            if len(t.r) > 64:
                t.r = t.r[-48:]
        for t in writes:
            t.w = tok
            t.r = []

    def op(self, e, fn, reads=(), writes=()):
        self._deps(e, reads, writes)
        ins = fn(self.eng[e])
        self.cnt[e] += 1
        ins.then_inc(self.sem[e], 1)
        self.ninstr += 1
        tok = (self.sem[e], self.cnt[e], e)
        self._record(tok, reads, writes)
        return tok

    def mm(self, fns, reads=(), writes=()):
        e = "pe"
        self._deps(e, reads, writes)
        ins = None
        for fn in fns:
            ins = fn(self.eng[e])
            self.ninstr += 1
        self.cnt[e] += 1
        ins.then_inc(self.sem[e], 1)
        tok = (self.sem[e], self.cnt[e], e)
        self._record(tok, reads, writes)
        return tok

    def dma(self, q, out_ap, in_ap, reads=(), writes=()):
        if q == "pool":
            i = self.dnext_sw
            self.dnext_sw = (self.dnext_sw + 1) % self.NSW
        else:
            i = self.NSW + self.dnext
            self.dnext = (self.dnext + 1) % (self.NDMA - self.NSW)
        if self.dcnt[i] > 0:
            self._wait(q, (self.dsem[i], self.dcnt[i], "dma"))
        for t in reads:
            self._wait(q, t.w)
        for t in writes:
            self._wait(q, t.w)
            for r in t.r:
                self._wait(q, r)
        self.dcnt[i] += 16
        self.eng[q].dma_start(out=out_ap, in_=in_ap).then_inc(self.dsem[i], 16)
        self.ninstr += 1
        tok = (self.dsem[i], self.dcnt[i], "dma")
        self._record(tok, reads, writes)
        return tok

    def barrier(self):
        toks = [(self.sem[e], self.cnt[e], e) for e in self.eng if self.cnt[e] > 0]
        toks += [(self.dsem[i], self.dcnt[i], "dma") for i in range(self.NDMA) if self.dcnt[i] > 0]
        for e in self.eng:
            for tok in toks:
                if tok[2] == e:
                    continue
                self._wait(e, tok)


def bc_mid(ap2d, n):
    N = ap2d.shape[-1]
    return ap2d.rearrange("p (o j) -> p o j", o=1).to_broadcast([128, n, N])


import os


def build_program(NT):
    NQB = NT * 4
    NH = NQB * 2
    NKT = NT * 2
    nc = bass.Bass("TRN2", target_bir_lowering=False)

    def din(name, shape, dt=F32):
        return nc.dram_tensor(name, list(shape), dt, kind="ExternalInput").ap()

    def dscr(name, shape, dt=BF16):
        return nc.dram_tensor(name, list(shape), dt, kind="Internal").ap()

    xT_own = din("xT_own", [NT, 128, 8, 512])
    xT_oth = din("xT_oth", [NT, 128, 8, 512])
    xT_halo = din("xT_halo", [128, 8, NH])
    pT_own = din("pT_own", [NT, 128, 2, 512])
    halo_valid = din("halo_valid", [128, NH])
    offrows_d = din("offrows", [128, 2, 512])
    tqk_d = din("tqk", [128, NQB * NT])
    limk_d = din("limk", [128, NQB])
    ident_d = din("ident", [128, 128])
    lnp_d = din("lnp", [128, 6 * 8])
    convw_d = din("convw", [128, 12])
    slopebig_d = din("slopebig", [128, 8])
    pow2_d = din("pow2", [128, NIT])
    wshapes = {
        "wg1": [FC, 128, 8, 128], "wu1": [FC, 128, 8, 128], "wd1": [8, 128, FC, 128],
        "wg2": [FC, 128, 8, 128], "wu2": [FC, 128, 8, 128], "wd2": [8, 128, FC, 128],
        "winst": [NCH, 128, 8, 128], "winv": [1, 128, 8, 512], "winwi": [1, 128, 8, 8],
        "wout": [8, 128, 8, 128], "wgate": [8, 128, 8, 128], "wproj": [8, 128, 2, 128],
    }
    w32 = {k: din(k, s) for k, s in wshapes.items()}
    wbf = {k: dscr(k + "_b", s) for k, s in wshapes.items()}
    wtok = {k: [T(None, "%s%d" % (k, i)) for i in range(s[0])] for k, s in wshapes.items()}
    KT_d = dscr("KT_d", [NKT, 128, 4, 512])
    V_d = dscr("V_d", [NKT, 128, 4, 512])
    X1_d = dscr("X1_d", [NT, 128, 8, 512], F32)
    Q_d = dscr("Q_d", [NT, 128, 4, 512])
    QI_d = dscr("QI_d", [NT, 128, 4, 512])
    WI_d = dscr("WI_d", [NT, 128, 32], F32)
    CONV_d = dscr("CONV_d", [NT, 128, 4, 512])
    ATT_d = dscr("ATT_d", [NT, 128, 4, 512])
    X1_tok = [T(None, "x1d%d" % i) for i in range(NT)]
    Q_tok = [T(None, "qd%d" % i) for i in range(NT)]
    QI_tok = [T(None, "qid%d" % i) for i in range(NT)]
    WI_tok = [T(None, "wid%d" % i) for i in range(NT)]
    CONV_tok = [T(None, "convd%d" % i) for i in range(NT)]
    ATT_tok = [T(None, "attd%d" % i) for i in range(NT)]
    KT_tok = [T(None, "ktd%d" % i) for i in range(NKT)]
    V_tok = [T(None, "vd%d" % i) for i in range(NKT)]
    outT = nc.dram_tensor("outT", [NT, 128, 8, 512], F32, kind="ExternalOutput").ap()
    out_tok = T(None, "out")

    with ExitStack() as st:
        fw = FW(nc, st)
        order = ["wg1", "wu1", "wd1", "winst", "winv", "winwi", "wout", "wg2", "wu2", "wd2", "wgate", "wproj"]
        for k in order:
            for i in range(wshapes[k][0]):
                fw.dma("pool", wbf[k][i], w32[k][i], writes=[wtok[k][i]])

        ki2 = fw.sb("ki2", [128, NKT * 512], BF16)
        ident32 = fw.sb("ident32", [128, 128], F32)
        identb = fw.sb("identb", [128, 128], BF16)
        ones32 = fw.sb("ones32", [128, 128], F32)
        onesb = fw.sb("onesb", [128, 128], BF16)
        lnp = fw.sb("lnp", [128, 48], F32)
        convw = fw.sb("convw", [128, 12], F32)
        slopebig = fw.sb("slopebig", [128, 8], F32)
        pow2 = fw.sb("pow2", [128, NIT], F32)
        offrows = fw.sb("offrows", [128, 2, 512], F32)
        tqk = fw.sb("tqk", [128, NQB * NT], F32)
        limk = fw.sb("limk", [128, NQB], F32)
        hval = fw.sb("hval", [128, NH], F32)
        zh = fw.sb("zh", [128, 4, NH], F32)
        bank = [fw.ps("bank%d" % i, [128, 512], F32) for i in range(7)]
        pbf_t = fw.ps("pbf", [128, 2, 512], BF16)
        pbfs = [T(pbf_t.h, "pbfA"), T(pbf_t.h, "pbfB")]
        PO = bank[6]

        for (t, d) in [(ident32, ident_d), (lnp, lnp_d), (convw, convw_d), (slopebig, slopebig_d), (pow2, pow2_d),
                       (offrows, offrows_d), (tqk, tqk_d), (limk, limk_d), (hval, halo_valid)]:
            fw.dma("sp", t[:], d, writes=[t])
        fw.op("dve", lambda e: e.tensor_copy(out=identb[:], in_=ident32[:]), reads=[ident32], writes=[identb])
        fw.op("dve", lambda e: e.memset(ones32[:], 1.0 / 1024.0), writes=[ones32])
        fw.op("dve", lambda e: e.memset(onesb[:], 1.0 / 1024.0), writes=[onesb])

        def alloc_ffn_scratch(stk, with_win):
            sc = {}
            sc["xb"] = fw.sb("xb", [128, 8, 512], BF16, stk)
            sc["hT"] = fw.sb("hT", [128, FC, 512], BF16, stk)
            sc["sg"] = [fw.sb("sg%d" % i, [128, 512], BF16, stk) for i in range(2)]
            sc["sq"] = fw.sb("sq", [128, 8, 512], BF16, stk)
            sc["mean"] = fw.sb("mean", [128, 512], F32, stk)
            sc["msq"] = fw.sb("msq", [128, 512], F32, stk)
            sc["rstd"] = fw.sb("rstd", [128, 512], F32, stk)
            sc["wgs"] = [fw.sb("wgs%d" % i, [128, 8, 128], BF16, stk) for i in range(2)]
            sc["wus"] = [fw.sb("wus%d" % i, [128, 8, 128], BF16, stk) for i in range(2)]
            sc["wds"] = [fw.sb("wds%d" % i, [128, FC, 128], BF16, stk) for i in range(2)]
            sc["wcs"] = [fw.sb("wcs%d" % i, [128, 8, 128], BF16, stk) for i in range(3)]
            sc["wci"] = 0
            sc["bki"] = 0
            sc["evi"] = 0
            if with_win:
                sc["wv_sb"] = fw.sb("wv_sb", [128, 8, 512], BF16, stk)
                sc["wwi_sb"] = fw.sb("wwi_sb", [128, 8, 8], BF16, stk)
                fw.dma("sp", sc["wv_sb"][:], wbf["winv"][0], reads=[wtok["winv"][0]], writes=[sc["wv_sb"]])
                fw.dma("sp", sc["wwi_sb"][:], wbf["winwi"][0], reads=[wtok["winwi"][0]], writes=[sc["wwi_sb"]])
                sc["cg_sb"] = fw.sb("cg_sb", [128, 512], F32, stk)
            return sc

        def load_chunk(sc, key, idx):
            s = sc["wcs"][sc["wci"] % 3]
            sc["wci"] += 1
            fw.dma("sp", s[:], wbf[key][idx], reads=[wtok[key][idx]], writes=[s])
            return s

        def nextbank(sc):
            b = bank[sc["bki"] % 6]
            sc["bki"] += 1
            return b

        def ffn(xt, N, kg, ku, kd, sc):
            hT, sg, xb = sc["hT"], sc["sg"], sc["xb"]
            wgs, wus, wds = sc["wgs"], sc["wus"], sc["wds"]

            def load_gu(c):
                fw.dma("sp", wgs[c % 2][:], wbf[kg][c], reads=[wtok[kg][c]], writes=[wgs[c % 2]])
                fw.dma("sp", wus[c % 2][:], wbf[ku][c], reads=[wtok[ku][c]], writes=[wus[c % 2]])

            def load_d(dc):
                fw.dma("sp", wds[dc % 2][:], wbf[kd][dc], reads=[wtok[kd][dc]], writes=[wds[dc % 2]])

            load_gu(0)
            for c in range(FC):
                if c + 1 < FC:
                    load_gu(c + 1)
                else:
                    load_d(0)
                pg, pu = bank[c % 2], bank[2 + c % 2]
                wg_, wu_ = wgs[c % 2], wus[c % 2]
                fw.mm([(lambda e, kc=kc: e.matmul(pg[:, :N], lhsT=wg_[:, kc, :], rhs=xb[:, kc, :N],
                                                  start=(kc == 0), stop=(kc == 7))) for kc in range(8)],
                      reads=[wg_, xb], writes=[pg])
                fw.mm([(lambda e, kc=kc: e.matmul(pu[:, :N], lhsT=wu_[:, kc, :], rhs=xb[:, kc, :N],
                                                  start=(kc == 0), stop=(kc == 7))) for kc in range(8)],
                      reads=[wu_, xb], writes=[pu])
                s_ = sg[c % 2]
                fw.op("act", lambda e: e.activation(out=s_[:, :N], in_=pg[:, :N], func=AF.Silu),
                      reads=[pg], writes=[s_])
                fw.op("dve", lambda e: e.tensor_tensor(out=hT[:, c, :N], in0=s_[:, :N], in1=pu[:, :N], op=ALU.mult),
                      reads=[s_, pu], writes=[hT])
            for dc in range(8):
                if dc + 1 < 8:
                    load_d(dc + 1)
                po = bank[4 + dc % 2]
                wd_ = wds[dc % 2]
                fw.mm([(lambda e, fc=fc: e.matmul(po[:, :N], lhsT=wd_[:, fc, :], rhs=hT[:, fc, :N],
                                                  start=(fc == 0), stop=(fc == FC - 1))) for fc in range(FC)],
                      reads=[wd_, hT], writes=[po])
                fw.op("dve", lambda e: e.scalar_tensor_tensor(out=xt[:, dc, :N], in0=xt[:, dc, :N],
                                                              scalar=2.0 * DN_ALPHA, in1=po[:, :N],
                                                              op0=ALU.mult, op1=ALU.add),
                      reads=[xt, po], writes=[xt])

        def layernorm(xt, N, li, eps, sc):
            sq, mean, msq, rstd, xb = sc["sq"], sc["mean"], sc["msq"], sc["rstd"], sc["xb"]
            fw.op("act", lambda e: e.activation(out=sq[:, :, :N], in_=xt[:, :, :N], func=AF.Square),
                  reads=[xt], writes=[sq])
            p1, p2 = bank[4], bank[5]
            fw.mm([(lambda e, kc=kc: e.matmul(p1[:, :N], lhsT=ones32[:], rhs=xt[:, kc, :N],
                                              start=(kc == 0), stop=(kc == 7))) for kc in range(8)],
                  reads=[ones32, xt], writes=[p1])
            fw.mm([(lambda e, kc=kc: e.matmul(p2[:, :N], lhsT=onesb[:], rhs=sq[:, kc, :N],
                                              start=(kc == 0), stop=(kc == 7))) for kc in range(8)],
                  reads=[onesb, sq], writes=[p2])
            fw.op("act", lambda e: e.copy(out=mean[:, :N], in_=p1[:, :N]), reads=[p1], writes=[mean])
            fw.op("dve", lambda e: e.tensor_tensor(out=msq[:, :N], in0=mean[:, :N], in1=mean[:, :N], op=ALU.mult),
                  reads=[mean], writes=[msq])
            fw.op("dve", lambda e: e.tensor_tensor(out=msq[:, :N], in0=p2[:, :N], in1=msq[:, :N], op=ALU.subtract),
                  reads=[p2, msq], writes=[msq])
            fw.op("dve", lambda e: e.tensor_scalar(out=msq[:, :N], in0=msq[:, :N], scalar1=eps, scalar2=None,
                                                   op0=ALU.add), reads=[msq], writes=[msq])
            fw.op("act", lambda e: e.activation(out=rstd[:, :N], in_=msq[:, :N], func=AF.Sqrt),
                  reads=[msq], writes=[rstd])
            fw.op("dve", lambda e: e.reciprocal(out=rstd[:, :N], in_=rstd[:, :N]), reads=[rstd], writes=[rstd])
            for hf in range(2):
                ks = slice(hf * 4, hf * 4 + 4)
                fw.op("dve", lambda e: e.tensor_tensor(out=xt[:, ks, :N], in0=xt[:, ks, :N], in1=bc_mid(mean[:, :N], 4),
                                                       op=ALU.subtract), reads=[xt, mean], writes=[xt])
                fw.op("dve", lambda e: e.tensor_tensor(out=xt[:, ks, :N], in0=xt[:, ks, :N], in1=bc_mid(rstd[:, :N], 4),
                                                       op=ALU.mult), reads=[xt, rstd], writes=[xt])
                for kc in range(hf * 4, hf * 4 + 4):
                    fw.op("act", lambda e, kc=kc: e.activation(out=xt[:, kc, :N], in_=xt[:, kc, :N], func=AF.Identity,
                                                                scale=lnp[:, (2 * li) * 8 + kc:(2 * li) * 8 + kc + 1],
                                                                bias=lnp[:, (2 * li + 1) * 8 + kc:(2 * li + 1) * 8 + kc + 1]),
                          reads=[xt, lnp], writes=[xt])
            fw.op("dve", lambda e: e.tensor_copy(out=xb[:, :, :N], in_=xt[:, :, :N]), reads=[xt], writes=[xb])

        def proj_st(sc, ci, N, n0=0):
            xb = sc["xb"]
            w_ = load_chunk(sc, "winst", ci)
            b = nextbank(sc)
            fw.mm([(lambda e, kc=kc: e.matmul(b[:, :N], lhsT=w_[:, kc, :], rhs=xb[:, kc, n0:n0 + N],
                                              start=(kc == 0), stop=(kc == 7))) for kc in range(8)],
                  reads=[w_, xb], writes=[b])
            return b

        def evac(sc, out_ap, in_ap, reads, writes, scale=None):
            sc["evi"] += 1
            if sc["evi"] % 2 == 0:
                if scale is None:
                    fw.op("act", lambda e: e.copy(out=out_ap, in_=in_ap), reads=reads, writes=writes)
                else:
                    fw.op("act", lambda e: e.mul(out=out_ap, in_=in_ap, mul=scale), reads=reads, writes=writes)
            else:
                if scale is None:
                    fw.op("dve", lambda e: e.tensor_copy(out=out_ap, in_=in_ap), reads=reads, writes=writes)
                else:
                    fw.op("dve", lambda e: e.tensor_scalar(out=out_ap, in0=in_ap, scalar1=scale, scalar2=None,
                                                           op0=ALU.mult), reads=reads, writes=writes)

        def conv_part(N, sc, own_T):
            cg_sb = sc["cg_sb"]
            for c in range(4):
                pcg = proj_st(sc, CH_CG + c, N)
                pu_ = proj_st(sc, CH_U + c, N)
                fw.op("act", lambda e: e.copy(out=cg_sb[:, :N], in_=pcg[:, :N]), reads=[pcg], writes=[cg_sb])
                if own_T is None:
                    fw.op("dve", lambda e: e.tensor_tensor(out=zh[:, c, :N], in0=cg_sb[:, :N], in1=pu_[:, :N],
                                                           op=ALU.mult), reads=[cg_sb, pu_], writes=[zh])
                    fw.op("dve", lambda e: e.tensor_tensor(out=zh[:, c, :N], in0=zh[:, c, :N], in1=hval[:, :N],
                                                           op=ALU.mult), reads=[zh, hval], writes=[zh])
                    continue
                z, acc = sc["z"], sc["acc"]
                pbg = proj_st(sc, CH_BG + c, N)
                fw.op("dve", lambda e: e.tensor_tensor(out=z[:, :, 2:130],
                                                       in0=cg_sb[:].rearrange("p (b j) -> p b j", b=4),
                                                       in1=pu_[:].rearrange("p (b j) -> p b j", b=4), op=ALU.mult),
                      reads=[cg_sb, pu_], writes=[z])
                fw.op("dve", lambda e: e.tensor_copy(
                    out=z[:, :, 0:2],
                    in_=zh[:, c, own_T * 8:(own_T + 1) * 8].rearrange("p (b k) -> p b k", k=2)),
                      reads=[zh, z], writes=[z])
                fw.op("dve", lambda e: e.tensor_scalar(out=acc[:], in0=z[:, :, 0:128],
                                                       scalar1=convw[:, c * 3 + 0:c * 3 + 1], scalar2=None,
                                                       op0=ALU.mult), reads=[z, convw], writes=[acc])
                for j in (1, 2):
                    fw.op("dve", lambda e, j=j: e.scalar_tensor_tensor(out=acc[:], in0=z[:, :, j:j + 128],
                                                                        scalar=convw[:, c * 3 + j:c * 3 + j + 1],
                                                                        in1=acc[:], op0=ALU.mult, op1=ALU.add),
                          reads=[z, convw, acc], writes=[acc])
                convT = sc["convT"]
                fw.op("dve", lambda e: e.tensor_tensor(out=convT[:, c, :].rearrange("p (b j) -> p b j", b=4),
                                                       in0=acc[:], in1=pbg[:].rearrange("p (b j) -> p b j", b=4),
                                                       op=ALU.mult), reads=[acc, pbg], writes=[convT])

        def kv_part(kt, sc):
            kt_sb, v_sb, xb, wv_sb = sc["kt_sb"], sc["v_sb"], sc["xb"], sc["wv_sb"]
            for j in range(4):
                b = proj_st(sc, CH_K + j, 512)
                evac(sc, kt_sb[:, j, :], b[:], [b], [kt_sb])
            fw.dma("sp", KT_d[kt], kt_sb[:], reads=[kt_sb], writes=[KT_tok[kt]])
            b = proj_st(sc, CH_KI, 512)
            evac(sc, ki2[:, kt * 512:(kt + 1) * 512], b[:], [b], [ki2])
            for blk in range(4):
                b = nextbank(sc)
                fw.mm([(lambda e, kc=kc: e.matmul(b[:], lhsT=xb[:, kc, blk * 128:(blk + 1) * 128], rhs=wv_sb[:, kc, :],
                                                  start=(kc == 0), stop=(kc == 7))) for kc in range(8)],
                      reads=[xb, wv_sb], writes=[b])
                evac(sc, v_sb[:, blk, :], b[:], [b], [v_sb])
            fw.dma("sp", V_d[kt], v_sb[:], reads=[v_sb], writes=[V_tok[kt]])

        def q_part(sc, Tt):
            xb, wwi_sb = sc["xb"], sc["wwi_sb"]
            qc, qic, wis = sc["qc"], sc["qic"], sc["wis"]
            for j in range(4):
                b = proj_st(sc, CH_Q + j, 512)
                evac(sc, qc[:, j, :], b[:], [b], [qc], scale=0.125)
            fw.dma("sp", Q_d[Tt], qc[:], reads=[qc], writes=[Q_tok[Tt]])
            for j in range(4):
                b = proj_st(sc, CH_QI + j, 512)
                evac(sc, qic[:, j, :], b[:], [b], [qic])
            fw.dma("sp", QI_d[Tt], qic[:], reads=[qic], writes=[QI_tok[Tt]])
            b = nextbank(sc)
            for blk in range(4):
                fw.mm([(lambda e, kc=kc: e.matmul(b[:, blk * 8:(blk + 1) * 8], lhsT=xb[:, kc, blk * 128:(blk + 1) * 128],
                                                  rhs=wwi_sb[:, kc, :], start=(kc == 0), stop=(kc == 7)))
                       for kc in range(8)], reads=[xb, wwi_sb], writes=[b])
            fw.op("dve", lambda e: e.tensor_scalar(out=wis[:], in0=b[:, 0:32], scalar1=IDX_SCALE, scalar2=None,
                                                   op0=ALU.mult), reads=[b], writes=[wis])
            fw.dma("sp", WI_d[Tt], wis[:], reads=[wis], writes=[WI_tok[Tt]])

        with fw.scope() as stk:
            sc = alloc_ffn_scratch(stk, True)
            xth = fw.sb("xth", [128, 8, 512], F32, stk)
            fw.dma("sp", xth[:, :, :NH], xT_halo, writes=[xth])
            fw.op("dve", lambda e: e.tensor_copy(out=sc["xb"][:, :, :NH], in_=xth[:, :, :NH]), reads=[xth], writes=[sc["xb"]])
            ffn(xth, NH, "wg1", "wu1", "wd1", sc)
            layernorm(xth, NH, 0, 4.0 * LN_EPS, sc)
            conv_part(NH, sc, None)

        with fw.scope() as stk:
            sc = alloc_ffn_scratch(stk, True)
            sc["z"] = fw.sb("z", [128, 4, 130], F32, stk)
            sc["acc"] = fw.sb("acc", [128, 4, 128], F32, stk)
            sc["kt_sb"] = fw.sb("kt_sb", [128, 4, 512], BF16, stk)
            sc["v_sb"] = fw.sb("v_sb", [128, 4, 512], BF16, stk)
            sc["qc"] = fw.sb("qc", [128, 4, 512], BF16, stk)
            sc["qic"] = fw.sb("qic", [128, 4, 512], BF16, stk)
            sc["wis"] = fw.sb("wis", [128, 32], F32, stk)
            sc["convT"] = fw.sb("convT", [128, 4, 512], BF16, stk)
            xts = [fw.sb("xts%d" % i, [128, 8, 512], F32, stk) for i in range(2)]
            xb = sc["xb"]
            for Tt in range(NT):
                for sub in (0, 1):
                    xt = xts[sub]
                    src = xT_own if sub == 0 else xT_oth
                    fw.dma("sp", xt[:], src[Tt], writes=[xt])
                    fw.op("dve", lambda e: e.tensor_copy(out=xb[:], in_=xt[:]), reads=[xt], writes=[xb])
                    ffn(xt, 512, "wg1", "wu1", "wd1", sc)
                    layernorm(xt, 512, 0, 4.0 * LN_EPS, sc)
                    if sub == 0:
                        fw.dma("sp", X1_d[Tt], xt[:], reads=[xt], writes=[X1_tok[Tt]])
                    kv_part(2 * Tt + sub, sc)
                    if sub == 0:
                        q_part(sc, Tt)
                        conv_part(512, sc, Tt)
                        fw.dma("sp", CONV_d[Tt], sc["convT"][:], reads=[sc["convT"]], writes=[CONV_tok[Tt]])

        with fw.scope() as stk:
            score = [fw.sb("score%d" % i, [128, NKT * 512], F32, stk) for i in range(2)]
            junk = fw.sb("junk", [128, NKT * 512], BF16, stk)
            Rr = [fw.sb("R%d" % i, [128, 512], BF16, stk) for i in range(3)]
            Tts = [fw.sb("Tt%d" % i, [128, 512], F32, stk) for i in range(3)]
            Pp = [fw.sb("P%d" % i, [128, 512], BF16, stk) for i in range(3)]
            PTs = [fw.sb("PT%d" % i, [128, 4, 128], BF16, stk) for i in range(3)]
            ktile = [fw.sb("ktile%d" % i, [128, 4, 512], BF16, stk) for i in range(2)]
            vtile = [fw.sb("vtile%d" % i, [128, 4, 512], BF16, stk) for i in range(2)]
            diag = fw.sb("diag", [128, 8, 128], BF16, stk)
            distn = [fw.sb("distn%d" % i, [128, 512], F32, stk) for i in range(2)]
            mb = fw.sb("mb", [128, 512], F32, stk)
            attn_sb = fw.sb("attn_sb", [128, 512], BF16, stk)
            amaxc = [fw.sb("amaxc%d" % i, [128, NKT], F32, stk) for i in range(2)]
            small = [fw.sb("small%d" % i, [128, 16], F32, stk) for i in range(2)]
            Wi = [fw.sb("Wi%d" % i, [128, NIT], F32, stk) for i in range(2)]
            cnt = [fw.sb("cnt%d" % i, [128, NIT * 4], F32, stk) for i in range(2)]
            rowsum = fw.sb("rowsum", [128, 8 * NKT], F32, stk)
            rs = fw.sb("rs", [128, 8], F32, stk)
            biasc = fw.sb("biasc", [128, 8], F32, stk)
            qXs = [[fw.sb("qX%d_%d" % (s_, i), [128, 4, 512], BF16, stk) for i in range(2)] for s_ in range(2)]
            qiXs = [[fw.sb("qiX%d_%d" % (s_, i), [128, 4, 512], BF16, stk) for i in range(2)] for s_ in range(2)]
            wi_sbs = [fw.sb("wi_sb%d" % s_, [128, 32], F32, stk) for s_ in range(2)]
            attT = [fw.sb("attT%d" % s_, [128, 4, 512], BF16, stk) for s_ in range(2)]
            for s_ in range(2):
                for t in qXs[s_] + qiXs[s_]:
                    fw.op("dve", lambda e, t=t: e.memset(t[:], 0.0), writes=[t])

            def load_tile_q(Tt):
                s_ = Tt % 2
                for eh in range(2):
                    ps = slice(eh * 64, (eh + 1) * 64)
                    fw.dma("sp", qXs[s_][eh][ps], Q_d[Tt][ps], reads=[Q_tok[Tt]], writes=[qXs[s_][eh]])
                    fw.dma("sp", qiXs[s_][eh][ps], QI_d[Tt][ps], reads=[QI_tok[Tt]], writes=[qiXs[s_][eh]])
                fw.dma("sp", wi_sbs[s_][:], WI_d[Tt], reads=[WI_tok[Tt]], writes=[wi_sbs[s_]])

            def gen_indexer(g):
                Tt, qb = divmod(g, 4)
                nkt = 2 * Tt + 2
                if qb == 0:
                    load_tile_q(Tt)
                par = g % 2
                qiX, wi_sb = qiXs[Tt % 2], wi_sbs[Tt % 2]
                qs = slice(qb * 128, (qb + 1) * 128)
                sco, amx = score[par], amaxc[par]
                for h in range(8):
                    fw.op("dve", lambda e, h=h: e.tensor_scalar(out=diag[:, h, :], in0=identb[:],
                                                                 scalar1=wi_sb[:, qb * 8 + h:qb * 8 + h + 1],
                                                                 scalar2=None, op0=ALU.mult),
                          reads=[identb, wi_sb], writes=[diag])
                items = [(kt, h) for kt in range(nkt) for h in range(8)]
                n = len(items)

                def A(i):
                    kt, h = items[i]
                    hp, eh = divmod(h, 2)
                    pr = bank[i % 3]
                    fw.mm([lambda e: e.matmul(pr[:], lhsT=qiX[eh][:, hp, qs], rhs=ki2[:, kt * 512:(kt + 1) * 512],
                                              start=True, stop=True)], reads=[qiX[eh], ki2], writes=[pr])

                def B(i):
                    pr, r_ = bank[i % 3], Rr[i % 3]
                    if items[i][1] < 5:
                        fw.op("act", lambda e: e.activation(out=r_[:], in_=pr[:], func=AF.Relu), reads=[pr], writes=[r_])
                    else:
                        fw.op("dve", lambda e: e.tensor_scalar(out=r_[:], in0=pr[:], scalar1=0.0, scalar2=None,
                                                               op0=ALU.max), reads=[pr], writes=[r_])

                def C(i):
                    kt, h = items[i]
                    psc, r_ = bank[3 + kt % 2], Rr[i % 3]
                    fw.mm([lambda e: e.matmul(psc[:], lhsT=diag[:, h, :], rhs=r_[:], start=(h == 0), stop=(h == 7))],
                          reads=[diag, r_], writes=[psc])
                    if h == 7:
                        sl = slice(kt * 512, (kt + 1) * 512)
                        fw.op("dve", lambda e: e.tensor_copy(out=sco[:, sl], in_=psc[:]), reads=[psc], writes=[sco])
                        fw.op("dve", lambda e: e.tensor_reduce(out=amx[:, kt:kt + 1], in_=sco[:, sl], axis=AX.X,
                                                               op=ALU.max, apply_absolute_value=True),
                              reads=[sco], writes=[amx])
                        if kt >= 2 * Tt:
                            sub = kt - 2 * Tt
                            fw.op("dve", lambda e: e.tensor_scalar(out=mb[:], in0=offrows[:, sub, :],
                                                                   scalar1=limk[:, g:g + 1], scalar2=NEG,
                                                                   op0=ALU.is_ge, op1=ALU.mult),
                                  reads=[offrows, limk], writes=[mb])
                            fw.op("dve", lambda e: e.tensor_tensor(out=sco[:, sl], in0=sco[:, sl], in1=mb[:],
                                                                   op=ALU.add), reads=[sco, mb], writes=[sco])
                A(0)
                A(1)
                for i in range(n):
                    if i + 2 < n:
                        A(i + 2)
                    B(i)
                    C(i)
                    if items[i][1] == 7:
                        yield

            def gen_bisect(g):
                Tt, qb = divmod(g, 4)
                nkt = 2 * Tt + 2
                L = nkt * 512
                par = g % 2
                sco, amx, sm, wi_, cn = score[par], amaxc[par], small[par], Wi[par], cnt[par]
                fw.op("dve", lambda e: e.memset(cn[:], 0.0), writes=[cn])
                fw.op("dve", lambda e: e.tensor_reduce(out=sm[:, 0:1], in_=amx[:, :nkt], axis=AX.X, op=ALU.max),
                      reads=[amx], writes=[sm])
                fw.op("dve", lambda e: e.tensor_scalar(out=sm[:, 1:2], in0=sm[:, 0:1], scalar1=2.02,
                                                       scalar2=2e-6, op0=ALU.mult, op1=ALU.add),
                      reads=[sm], writes=[sm])
                fw.op("dve", lambda e: e.tensor_scalar(out=wi_[:], in0=pow2[:], scalar1=sm[:, 1:2], scalar2=None,
                                                       op0=ALU.mult), reads=[pow2, sm], writes=[wi_])
                fw.op("dve", lambda e: e.memset(sm[:, 2:3], 0.0), reads=[sm], writes=[sm])
                CH = 2048
                nch = (L + CH - 1) // CH
                for it in range(NIT):
                    for c in range(nch):
                        c0, c1 = c * CH, min(L, (c + 1) * CH)
                        fw.op("dve", lambda e: e.tensor_scalar(out=junk[:, c0:c1], in0=sco[:, c0:c1],
                                                               scalar1=sm[:, 2:3], scalar2=0.0,
                                                               op0=ALU.is_ge, op1=ALU.add,
                                                               accum_out=cn[:, it * 4 + c:it * 4 + c + 1]),
                              reads=[sco, sm, cn], writes=[junk, cn])
                        if c + 1 < nch:
                            yield
                    if nch > 1:
                        fw.op("dve", lambda e: e.tensor_reduce(out=sm[:, 6:7], in_=cn[:, it * 4:it * 4 + nch], axis=AX.X,
                                                               op=ALU.add), reads=[cn, sm], writes=[sm])
                        csrc = sm[:, 6:7]
                    else:
                        csrc = cn[:, it * 4:it * 4 + 1]
                    fw.op("dve", lambda e: e.tensor_scalar(out=sm[:, 3:4], in0=csrc,
                                                           scalar1=float(TOPK), scalar2=-0.5,
                                                           op0=ALU.is_ge, op1=ALU.add),
                          reads=[cn, sm], writes=[sm])
                    fw.op("dve", lambda e: e.scalar_tensor_tensor(out=sm[:, 2:3], in0=sm[:, 3:4],
                                                                  scalar=wi_[:, it:it + 1], in1=sm[:, 2:3],
                                                                  op0=ALU.mult, op1=ALU.add),
                          reads=[sm, wi_], writes=[sm])
                    yield
                fw.op("dve", lambda e: e.scalar_tensor_tensor(out=sm[:, 4:5], in0=wi_[:, NIT - 1:NIT], scalar=-0.5,
                                                              in1=sm[:, 2:3], op0=ALU.mult, op1=ALU.add),
                      reads=[sm, wi_], writes=[sm])

            def gen_estep(g):
                Tt, qb = divmod(g, 4)
                nkt = 2 * Tt + 2
                L = nkt * 512
                par = g % 2
                sco, sm = score[par], small[par]
                for kt in range(nkt):
                    Tk, sub = divmod(kt, 2)
                    dn = distn[kt % 2]
                    sl = slice(kt * 512, (kt + 1) * 512)
                    fw.op("act", lambda e: e.activation(out=dn[:], in_=offrows[:, sub, :], func=AF.Abs,
                                                        bias=tqk[:, g * NT + Tk:g * NT + Tk + 1], scale=1.0),
                          reads=[offrows, tqk], writes=[dn])
                    fw.op("dve", lambda e: e.scalar_tensor_tensor(out=sco[:, sl], in0=sco[:, sl],
                                                                  scalar=sm[:, 4:5], in1=dn[:],
                                                                  op0=ALU.is_lt, op1=ALU.add),
                          reads=[sco, sm, dn], writes=[sco])
                    yield
                fw.op("dve", lambda e: e.tensor_reduce(out=sm[:, 5:6], in_=sco[:, :L], axis=AX.X, op=ALU.min),
                      reads=[sco], writes=[sm])
                fw.op("dve", lambda e: e.tensor_scalar(out=biasc[:], in0=slopebig[:], scalar1=sm[:, 5:6],
                                                       scalar2=None, op0=ALU.mult),
                      reads=[slopebig, sm], writes=[biasc])

            def gen_attn(g):
                Tt, qb = divmod(g, 4)
                nkt = 2 * Tt + 2
                par = g % 2
                qX = qXs[Tt % 2]
                at_ = attT[Tt % 2]
                qs = slice(qb * 128, (qb + 1) * 128)
                sco = score[par]
                fw.op("dve", lambda e: e.memset(rowsum[:], 0.0), writes=[rowsum])
                items = [(kt, h) for kt in range(nkt) for h in range(8)]
                n = len(items)

                def load_kv(kt):
                    fw.dma("sp", ktile[kt % 2][:], KT_d[kt], reads=[KT_tok[kt]], writes=[ktile[kt % 2]])
                    fw.dma("sp", vtile[kt % 2][:], V_d[kt], reads=[V_tok[kt]], writes=[vtile[kt % 2]])

                def A(i):
                    kt, h = items[i]
                    hp, eh = divmod(h, 2)
                    ps_, kt_ = bank[i % 3], ktile[kt % 2]
                    fw.mm([lambda e: e.matmul(ps_[:], lhsT=qX[eh][:, hp, qs], rhs=kt_[:, hp, :], start=True, stop=True)],
                          reads=[qX[eh], kt_], writes=[ps_])

                def BCDE(i):
                    kt, h = items[i]
                    sl = slice(kt * 512, (kt + 1) * 512)
                    ps_, t_, p_, pt_ = bank[i % 3], Tts[i % 3], Pp[i % 3], PTs[i % 3]
                    hb = i % 2
                    pbf = pbfs[hb]
                    fw.op("dve", lambda e: e.scalar_tensor_tensor(out=t_[:], in0=sco[:, sl], scalar=-SLOPES[h] * BIG,
                                                                  in1=ps_[:], op0=ALU.mult, op1=ALU.add),
                          reads=[sco, ps_], writes=[t_])
                    fw.op("act", lambda e: e.activation(out=p_[:], in_=t_[:], func=AF.Exp, bias=biasc[:, h:h + 1], scale=1.0,
                                                        accum_out=rowsum[:, h * NKT + kt:h * NKT + kt + 1]),
                          reads=[t_, biasc, rowsum], writes=[p_])
                    fw.mm([(lambda e, sbk=sbk: e.transpose(out=pbf[:, hb, sbk * 128:(sbk + 1) * 128],
                                                           in_=p_[:, sbk * 128:(sbk + 1) * 128], identity=identb[:]))
                           for sbk in range(4)], reads=[p_, identb], writes=[pbf])
                    src_ap = pbf[:, hb, :].rearrange("p (s q) -> p s q", s=4)
                    fw.op("act", lambda e: e.copy(out=pt_[:], in_=src_ap), reads=[pbf], writes=[pt_])

                def Fm(i):
                    kt, h = items[i]
                    pt_, vt_ = PTs[i % 3], vtile[kt % 2]
                    fw.mm([(lambda e, sbk=sbk: e.matmul(PO[:, h * 64:(h + 1) * 64], lhsT=pt_[:, sbk, :],
                                                        rhs=vt_[:, sbk, h * 64:(h + 1) * 64],
                                                        start=(i == 0 and sbk == 0), stop=(i == n - 1 and sbk == 3)))
                           for sbk in range(4)], reads=[pt_, vt_], writes=[PO])
                load_kv(0)
                if nkt > 1:
                    load_kv(1)
                A(0)
                A(1)
                for i in range(n):
                    if i + 2 < n:
                        A(i + 2)
                    BCDE(i)
                    if i >= 1:
                        Fm(i - 1)
                        kt_prev, h_prev = items[i - 1]
                        if h_prev == 7 and kt_prev + 2 < nkt:
                            load_kv(kt_prev + 2)
                    yield
                Fm(n - 1)
                fw.op("dve", lambda e: e.tensor_reduce(out=rs[:], in_=rowsum[:].rearrange("p (h k) -> p h k", k=NKT),
                                                       axis=AX.X, op=ALU.add), reads=[rowsum], writes=[rs, rowsum])
                fw.op("dve", lambda e: e.reciprocal(out=rs[:], in_=rs[:]), reads=[rs], writes=[rs])
                fw.op("dve", lambda e: e.tensor_tensor(
                    out=attn_sb[:].rearrange("p (h d) -> p h d", d=64),
                    in0=PO[:].rearrange("p (h d) -> p h d", d=64),
                    in1=rs[:].rearrange("p (h o) -> p h o", o=1).to_broadcast([128, 8, 64]), op=ALU.mult),
                      reads=[PO, rs], writes=[attn_sb])
                pbf = pbfs[0]
                fw.mm([(lambda e, c=c: e.transpose(out=pbf[:, 0, c * 128:(c + 1) * 128],
                                                   in_=attn_sb[:, c * 128:(c + 1) * 128], identity=identb[:]))
                       for c in range(4)], reads=[attn_sb, identb], writes=[pbf])
                fw.op("act", lambda e: e.copy(out=at_[:, :, qs],
                                              in_=pbf[:, 0, :].rearrange("p (c q) -> p c q", c=4)),
                      reads=[pbf], writes=[at_])
                if qb == 3:
                    fw.dma("sp", ATT_d[Tt], at_[:], reads=[at_], writes=[ATT_tok[Tt]])

            def run_pair(ga, na, gb, nb):
                a_alive, b_alive = ga is not None, gb is not None
                ia = ib = 0
                while a_alive or b_alive:
                    pa = (ia / na) if a_alive else 2.0
                    pb = (ib / nb) if b_alive else 2.0
                    if pa <= pb:
                        try:
                            next(ga)
                            ia += 1
                        except StopIteration:
                            a_alive = False
                    else:
                        try:
                            next(gb)
                            ib += 1
                        except StopIteration:
                            b_alive = False

            def nkt_of(g):
                return 2 * (g // 4) + 2

            for _ in gen_indexer(0):
                pass
            for g in range(NQB):
                run_pair(gen_bisect(g), NIT * ((nkt_of(g) * 512 + 2047) // 2048), gen_attn(g - 1) if g >= 1 else None,
                         8 * nkt_of(max(g - 1, 0)))
                run_pair(gen_indexer(g + 1) if g + 1 < NQB else None, nkt_of(min(g + 1, NQB - 1)),
                         gen_estep(g), nkt_of(g))
            for _ in gen_attn(NQB - 1):
                pass

        with fw.scope() as stk:
            sc = alloc_ffn_scratch(stk, False)
            xb = sc["xb"]
            x1s = [fw.sb("x1s%d" % i, [128, 8, 512], F32, stk) for i in range(2)]
            cats = [fw.sb("cats%d" % i, [128, 8, 512], BF16, stk) for i in range(2)]
            pt32s = [fw.sb("pt32_%d" % i, [128, 2, 512], F32, stk) for i in range(2)]
            pb = fw.sb("pb", [128, 2, 512], BF16, stk)
            sgm = [fw.sb("sgm%d" % i, [128, 512], F32, stk) for i in range(2)]
            wps = [fw.sb("wps%d" % i, [128, 2, 128], BF16, stk) for i in range(2)]

            def p3_loads(Tt):
                s_ = Tt % 2
                fw.dma("sp", x1s[s_][:], X1_d[Tt], reads=[X1_tok[Tt]], writes=[x1s[s_]])
                fw.dma("sp", cats[s_][:, 0:4, :], ATT_d[Tt], reads=[ATT_tok[Tt]], writes=[cats[s_]])
                fw.dma("sp", cats[s_][:, 4:8, :], CONV_d[Tt], reads=[CONV_tok[Tt]], writes=[cats[s_]])
                fw.dma("sp", pt32s[s_][:], pT_own[Tt], writes=[pt32s[s_]])
            p3_loads(0)
            for Tt in range(NT):
                if Tt + 1 < NT:
                    p3_loads(Tt + 1)
                x1o, catT, pt32 = x1s[Tt % 2], cats[Tt % 2], pt32s[Tt % 2]
                fw.op("dve", lambda e: e.tensor_copy(out=pb[:], in_=pt32[:]), reads=[pt32], writes=[pb])
                for dc in range(8):
                    w_ = load_chunk(sc, "wout", dc)
                    b = nextbank(sc)
                    fw.mm([(lambda e, fc=fc: e.matmul(b[:], lhsT=w_[:, fc, :], rhs=catT[:, fc, :],
                                                      start=(fc == 0), stop=(fc == 7))) for fc in range(8)],
                          reads=[w_, catT], writes=[b])
                    fw.op("dve", lambda e: e.scalar_tensor_tensor(out=x1o[:, dc, :], in0=x1o[:, dc, :], scalar=DN_ALPHA,
                                                                  in1=b[:], op0=ALU.mult, op1=ALU.add),
                          reads=[x1o, b], writes=[x1o])
                layernorm(x1o, 512, 1, LN_EPS, sc)
                ffn(x1o, 512, "wg2", "wu2", "wd2", sc)
                layernorm(x1o, 512, 2, 4.0 * LN_EPS, sc)
                for dc in range(8):
                    w_ = load_chunk(sc, "wgate", dc)
                    wp_ = wps[dc % 2]
                    sg_ = sgm[dc % 2]
                    fw.dma("sp", wp_[:], wbf["wproj"][dc], reads=[wtok["wproj"][dc]], writes=[wp_])
                    bg_ = nextbank(sc)
                    bp_ = nextbank(sc)
                    fw.mm([(lambda e, fc=fc: e.matmul(bg_[:], lhsT=w_[:, fc, :], rhs=xb[:, fc, :],
                                                      start=(fc == 0), stop=(fc == 7))) for fc in range(8)],
                          reads=[w_, xb], writes=[bg_])
                    fw.mm([(lambda e, fc=fc: e.matmul(bp_[:], lhsT=wp_[:, fc, :], rhs=pb[:, fc, :],
                                                      start=(fc == 0), stop=(fc == 1))) for fc in range(2)],
                          reads=[wp_, pb], writes=[bp_])
                    fw.op("act", lambda e: e.activation(out=sg_[:], in_=bg_[:], func=AF.Sigmoid), reads=[bg_], writes=[sg_])
                    fw.op("dve", lambda e: e.tensor_tensor(out=sg_[:], in0=sg_[:], in1=bp_[:], op=ALU.mult),
                          reads=[sg_, bp_], writes=[sg_])
                    fw.op("dve", lambda e: e.tensor_tensor(out=x1o[:, dc, :], in0=x1o[:, dc, :], in1=sg_[:], op=ALU.add),
                          reads=[x1o, sg_], writes=[x1o])
                fw.dma("sp", outT[Tt], x1o[:], reads=[x1o], writes=[out_tok])
        fw.barrier()
    return nc, fw


def _tile_w(W, kc, cc):
    return np.ascontiguousarray(W.reshape(kc, 128, cc, 128).transpose(2, 1, 0, 3))


def prep_weights(inp):
    f = np.float32
    w = {}
    w["wg1"] = _tile_w(inp["ffn1_wg"][0], 8, FC)
    w["wu1"] = _tile_w(inp["ffn1_wu"][0], 8, FC)
    w["wd1"] = _tile_w(inp["ffn1_wd"][0], FC, 8)
    w["wg2"] = _tile_w(inp["ffn2_wg"][0], 8, FC)
    w["wu2"] = _tile_w(inp["ffn2_wu"][0], 8, FC)
    w["wd2"] = _tile_w(inp["ffn2_wd"][0], FC, 8)
    win = inp["w_in"][0]
    cols = {"q": (0, 512), "k": (512, 1024), "v": (1024, 1536), "qi": (1536, 2048), "ki": (2048, 2112),
            "wi": (2112, 2120), "bg": (2120, 2632), "cg": (2632, 3144), "u": (3144, 3656)}

    def sub(n):
        a, b = cols[n]
        return win[:, a:b]
    st = np.concatenate([sub("k"), sub("ki"), sub("ki"), sub("q"), sub("qi"), sub("bg"), sub("cg"), sub("u")], axis=1)
    assert st.shape[1] == NCH * 128
    w["winst"] = _tile_w(st, 8, NCH)
    w["winv"] = np.ascontiguousarray(sub("v").reshape(8, 128, 512).transpose(1, 0, 2))[None]
    w["winwi"] = np.ascontiguousarray(sub("wi").reshape(8, 128, 8).transpose(1, 0, 2))[None]
    w["wout"] = _tile_w(inp["w_out"][0], 8, 8)
    w["wgate"] = _tile_w(inp["ple_gate_w"][0], 8, 8)
    w["wproj"] = _tile_w(inp["ple_proj_w"][0], 2, 8)
    lnp = np.stack([inp["ln1_g"][0], inp["ln1_b"][0], inp["ln2_g"][0], inp["ln2_b"][0],
                    inp["ln3_g"][0], inp["ln3_b"][0]], 0)
    w["lnp"] = np.ascontiguousarray(lnp.reshape(6, 8, 128).transpose(2, 0, 1).reshape(128, 48))
    cw = inp["conv_w"][0]
    w["convw"] = np.ascontiguousarray(cw.reshape(3, 4, 128).transpose(2, 1, 0).reshape(128, 12))
    w["ident"] = np.eye(128, dtype=f)
    w["slopebig"] = np.tile(np.array([s * BIG for s in SLOPES], dtype=f)[None, :], (128, 1))
    w["pow2"] = np.tile(np.array([2.0 ** (-(i + 1)) for i in range(NIT)], dtype=f)[None, :], (128, 1))
    return {k: np.ascontiguousarray(v, dtype=f) for k, v in w.items()}


def core_blocks(NT, role):
    own, oth = [], []
    for t in range(NT):
        b = 8 * t
        a = [b, b + 3, b + 4, b + 7]
        o = [b + 1, b + 2, b + 5, b + 6]
        if role == 0:
            own.append(a); oth.append(o)
        else:
            own.append(o); oth.append(a)
    return own, oth


def prep_core(x_b, p_b, NT, role):
    f = np.float32
    own, oth = core_blocks(NT, role)
    ar = np.arange(128)

    def pos_of(blocks):
        return np.concatenate([b * 128 + ar for b in blocks])
    own_pos = [pos_of(b) for b in own]
    oth_pos = [pos_of(b) for b in oth]

    def featmajor(rows, kc):
        return np.ascontiguousarray(rows.T.reshape(kc, 128, rows.shape[0]).transpose(1, 0, 2))
    m = {}
    m["xT_own"] = np.stack([featmajor(x_b[p], 8) for p in own_pos], 0)
    m["xT_oth"] = np.stack([featmajor(x_b[p], 8) for p in oth_pos], 0)
    m["pT_own"] = np.stack([featmajor(p_b[p], 2) for p in own_pos], 0)
    hpos, hval = [], []
    for t in range(NT):
        for b in own[t]:
            s = b * 128
            if s == 0:
                hpos += [0, 1]; hval += [0.0, 0.0]
            else:
                hpos += [s - 2, s - 1]; hval += [1.0, 1.0]
    m["xT_halo"] = featmajor(x_b[np.array(hpos)], 8)
    m["halo_valid"] = np.tile(np.array(hval, dtype=f)[None, :], (128, 1))
    off = np.stack([own_pos[0], oth_pos[0]], 0).astype(np.float64)
    m["offrows"] = np.tile((off * PSC)[None], (128, 1, 1))
    NQB = NT * 4
    tqk = np.zeros((128, NQB, NT), dtype=np.float64)
    limk = np.zeros((128, NQB), dtype=np.float64)
    for t in range(NT):
        for qb in range(4):
            pq = own_pos[t][qb * 128:(qb + 1) * 128].astype(np.float64)
            for tk in range(NT):
                tqk[:, t * 4 + qb, tk] = -(pq - 1024.0 * tk) * PSC
            lim = (np.floor(pq / 64) + 1) * 64
            limk[:, t * 4 + qb] = (lim - 1024.0 * t) * PSC
    m["tqk"] = tqk.reshape(128, NQB * NT)
    m["limk"] = limk
    return {k: np.ascontiguousarray(v, dtype=f) for k, v in m.items()}, own_pos


_CACHE = {}


def run_module(inputs, NT, B):
    x = np.asarray(inputs["x"], dtype=np.float32)
    p = np.asarray(inputs["p"], dtype=np.float32)[0]
    wts = prep_weights({k: np.asarray(v, dtype=np.float32) for k, v in inputs.items() if k not in ("x", "p")})
    in_maps, poss = [], []
    for b in range(B):
        for role in (0, 1):
            m, own_pos = prep_core(x[b], p[b], NT, role)
            m.update(wts)
            in_maps.append(m)
            poss.append((b, own_pos))
    if NT not in _CACHE:
        _CACHE[NT] = build_program(NT)
    nc, fw = _CACHE[NT]
    import time as _t
    _t0 = _t.time()
    res = run_bass_kernel_spmd(nc, in_maps, core_ids=list(range(2 * B)), trace=bool(os.environ.get('KTRACE')))
    if os.environ.get('KTRACE'):
        print('exec_time_ns', res.exec_time_ns, flush=True)
    if os.environ.get('KVERB'):
        print("ninstr", fw.ninstr, "spmd run s", _t.time() - _t0, flush=True)
    if os.environ.get('KRAW'):
        return [np.asarray(r["outT"]) for r in res.results]
    out = np.zeros((B, NT * 1024, D), dtype=np.float32)
    for ci, (b, own_pos) in enumerate(poss):
        oT = np.asarray(res.results[ci]["outT"])
        for t in range(NT):
            rows = oT[t].transpose(2, 1, 0).reshape(512, D)
            out[b, own_pos[t]] = rows
    return out


def kernel(**inputs):
    return run_module(inputs, 8, 4)
```

```python
import numpy as np
from contextlib import ExitStack, contextmanager
import concourse.bass as bass
import concourse.mybir as mybir
from concourse.bass_utils import run_bass_kernel_spmd

F32 = mybir.dt.float32
BF16 = mybir.dt.bfloat16
ALU = mybir.AluOpType
AF = mybir.ActivationFunctionType
AX = mybir.AxisListType

D = 1024
KC = 8
DFF = 2816
FC = 22
DN_ALPHA = 2.0 ** 0.25
IDX_SCALE = (8 ** -0.5) * (64 ** -0.5)
LN_EPS = 1e-5
TOPK = 256
BIG = float(2 ** 20)
PSC = 2.0 ** -20
NEG = -1.0e30
NIT = 14
SLOPES = [2.0 ** (-(h + 1)) for h in range(8)]
CH_K, CH_KI, CH_Q, CH_QI, CH_BG, CH_CG, CH_U = 0, 4, 5, 9, 13, 17, 21
NCH = 25


class T:
    __slots__ = ("h", "name", "w", "r")

    def __init__(self, h, name=""):
        self.h = h
        self.name = name
        self.w = None
        self.r = []

    def __getitem__(self, k):
        return self.h[k]


class FW:
    NDMA = 32
    NSW = 8

    def __init__(self, nc, stack):
        self.nc = nc
        self.stack = stack
        self.eng = {"pe": nc.tensor, "act": nc.scalar, "dve": nc.vector, "pool": nc.gpsimd, "sp": nc.sync}
        self.sem = {}
        self.cnt = {}
        self.seen = {e: {} for e in self.eng}
        for e in self.eng:
            self.sem[e] = stack.enter_context(nc.semaphore("s_" + e))
            self.cnt[e] = 0
        self.dsem = [stack.enter_context(nc.semaphore("d%d" % i)) for i in range(self.NDMA)]
        self.dcnt = [0] * self.NDMA
        self.dnext = 0
        self.dnext_sw = 0
        self.ninstr = 0
        self.uid = 0

    def sb(self, name, shape, dt, stack=None):
        st = stack or self.stack
        self.uid += 1
        return T(st.enter_context(self.nc.sbuf_tensor("%s_%d" % (name, self.uid), list(shape), dt)), name)

    def ps(self, name, shape, dt, stack=None):
        st = stack or self.stack
        self.uid += 1
        return T(st.enter_context(self.nc.psum_tensor("%s_%d" % (name, self.uid), list(shape), dt)), name)

    @contextmanager
    def scope(self):
        with ExitStack() as st:
            yield st
            self.barrier()

    def _wait(self, e, tok):
        if tok is None:
            return
        sem, val, src = tok
        key = id(sem)
        if self.seen[e].get(key, 0) >= val:
            return
        self.eng[e].wait_ge(sem, val)
        self.ninstr += 1
        self.seen[e][key] = val

    def _deps(self, e, reads, writes):
        for t in reads:
            self._wait(e, t.w)
        for t in writes:
            if not (e == "pe" and t.w is not None and t.w[2] == "pe"):
                self._wait(e, t.w)
            for r in t.r:
                if r[2] == e:
                    continue
                self._wait(e, r)

    def _record(self, tok, reads, writes):
        for t in reads:
            t.r.append(tok)
            if len(t.r) > 64:
                t.r = t.r[-48:]
        for t in writes:
            t.w = tok
            t.r = []

    def op(self, e, fn, reads=(), writes=()):
        self._deps(e, reads, writes)
        ins = fn(self.eng[e])
        self.cnt[e] += 1
        ins.then_inc(self.sem[e], 1)
        self.ninstr += 1
        tok = (self.sem[e], self.cnt[e], e)
        self._record(tok, reads, writes)
        return tok

    def mm(self, fns, reads=(), writes=()):
        e = "pe"
        self._deps(e, reads, writes)
        ins = None
        for fn in fns:
            ins = fn(self.eng[e])
            self.ninstr += 1
        self.cnt[e] += 1
        ins.then_inc(self.sem[e], 1)
        tok = (self.sem[e], self.cnt[e], e)
        self._record(tok, reads, writes)
        return tok

    def dma(self, q, out_ap, in_ap, reads=(), writes=()):
        if q == "pool":
            i = self.dnext_sw
            self.dnext_sw = (self.dnext_sw + 1) % self.NSW
        else:
            i = self.NSW + self.dnext
            self.dnext = (self.dnext + 1) % (self.NDMA - self.NSW)
        if self.dcnt[i] > 0:
            self._wait(q, (self.dsem[i], self.dcnt[i], "dma"))
        for t in reads:
            self._wait(q, t.w)
        for t in writes:
            self._wait(q, t.w)
            for r in t.r:
                self._wait(q, r)
        self.dcnt[i] += 16
        self.eng[q].dma_start(out=out_ap, in_=in_ap).then_inc(self.dsem[i], 16)
        self.ninstr += 1
        tok = (self.dsem[i], self.dcnt[i], "dma")
        self._record(tok, reads, writes)
        return tok

    def barrier(self):
        toks = [(self.sem[e], self.cnt[e], e) for e in self.eng if self.cnt[e] > 0]
        toks += [(self.dsem[i], self.dcnt[i], "dma") for i in range(self.NDMA) if self.dcnt[i] > 0]
        for e in self.eng:
            for tok in toks:
                if tok[2] == e:
                    continue
                self._wait(e, tok)


def bc_mid(ap2d, n):
    N = ap2d.shape[-1]
    return ap2d.rearrange("p (o j) -> p o j", o=1).to_broadcast([128, n, N])


import os


def build_program(NT):
    NQB = NT * 4
    NH = NQB * 2
    NKT = NT * 2
    nc = bass.Bass("TRN2", target_bir_lowering=False)

    def din(name, shape, dt=F32):
        return nc.dram_tensor(name, list(shape), dt, kind="ExternalInput").ap()

    def dscr(name, shape, dt=BF16):
        return nc.dram_tensor(name, list(shape), dt, kind="Internal").ap()

    xT_own = din("xT_own", [NT, 128, 8, 512])
    xT_oth = din("xT_oth", [NT, 128, 8, 512])
    xT_halo = din("xT_halo", [128, 8, NH])
    pT_own = din("pT_own", [NT, 128, 2, 512])
    halo_valid = din("halo_valid", [128, NH])
    offrows_d = din("offrows", [128, 2, 512])
    tqk_d = din("tqk", [128, NQB * NT])
    limk_d = din("limk", [128, NQB])
    ident_d = din("ident", [128, 128])
    lnp_d = din("lnp", [128, 6 * 8])
    convw_d = din("convw", [128, 12])
    slopebig_d = din("slopebig", [128, 8])
    pow2_d = din("pow2", [128, NIT])
    wshapes = {
        "wg1": [FC, 128, 8, 128], "wu1": [FC, 128, 8, 128], "wd1": [8, 128, FC, 128],
        "wg2": [FC, 128, 8, 128], "wu2": [FC, 128, 8, 128], "wd2": [8, 128, FC, 128],
        "winst": [NCH, 128, 8, 128], "winv": [1, 128, 8, 512], "winwi": [1, 128, 8, 8],
        "wout": [8, 128, 8, 128], "wgate": [8, 128, 8, 128], "wproj": [8, 128, 2, 128],
    }
    w32 = {k: din(k, s) for k, s in wshapes.items()}
    wbf = {k: dscr(k + "_b", s) for k, s in wshapes.items()}
    wtok = {k: [T(None, "%s%d" % (k, i)) for i in range(s[0])] for k, s in wshapes.items()}
    KT_d = dscr("KT_d", [NKT, 128, 4, 512])
    V_d = dscr("V_d", [NKT, 128, 4, 512])
    X1_d = dscr("X1_d", [NT, 128, 8, 512], F32)
    Q_d = dscr("Q_d", [NT, 128, 4, 512])
    QI_d = dscr("QI_d", [NT, 128, 4, 512])
    WI_d = dscr("WI_d", [NT, 128, 32], F32)
    CONV_d = dscr("CONV_d", [NT, 128, 4, 512])
    ATT_d = dscr("ATT_d", [NT, 128, 4, 512])
    X1_tok = [T(None, "x1d%d" % i) for i in range(NT)]
    Q_tok = [T(None, "qd%d" % i) for i in range(NT)]
    QI_tok = [T(None, "qid%d" % i) for i in range(NT)]
    WI_tok = [T(None, "wid%d" % i) for i in range(NT)]
    CONV_tok = [T(None, "convd%d" % i) for i in range(NT)]
    ATT_tok = [T(None, "attd%d" % i) for i in range(NT)]
    KT_tok = [T(None, "ktd%d" % i) for i in range(NKT)]
    V_tok = [T(None, "vd%d" % i) for i in range(NKT)]
    outT = nc.dram_tensor("outT", [NT, 128, 8, 512], F32, kind="ExternalOutput").ap()
    out_tok = T(None, "out")

    with ExitStack() as st:
        fw = FW(nc, st)
        order = ["wg1", "wu1", "wd1", "winst", "winv", "winwi", "wout", "wg2", "wu2", "wd2", "wgate", "wproj"]
        for k in order:
            for i in range(wshapes[k][0]):
                fw.dma("pool", wbf[k][i], w32[k][i], writes=[wtok[k][i]])

        ki2 = fw.sb("ki2", [128, NKT * 512], BF16)
        ident32 = fw.sb("ident32", [128, 128], F32)
        identb = fw.sb("identb", [128, 128], BF16)
        ones32 = fw.sb("ones32", [128, 128], F32)
        onesb = fw.sb("onesb", [128, 128], BF16)
        lnp = fw.sb("lnp", [128, 48], F32)
        convw = fw.sb("convw", [128, 12], F32)
        slopebig = fw.sb("slopebig", [128, 8], F32)
        pow2 = fw.sb("pow2", [128, NIT], F32)
        offrows = fw.sb("offrows", [128, 2, 512], F32)
        tqk = fw.sb("tqk", [128, NQB * NT], F32)
        limk = fw.sb("limk", [128, NQB], F32)
        hval = fw.sb("hval", [128, NH], F32)
        zh = fw.sb("zh", [128, 4, NH], F32)
        bank = [fw.ps("bank%d" % i, [128, 512], F32) for i in range(6)]
        pbfs = [fw.ps("pbfA", [128, 2, 512], BF16), fw.ps("pbfB", [128, 2, 512], BF16)]
        PO = bank[5]

        for (t, d) in [(ident32, ident_d), (lnp, lnp_d), (convw, convw_d), (slopebig, slopebig_d), (pow2, pow2_d),
                       (offrows, offrows_d), (tqk, tqk_d), (limk, limk_d), (hval, halo_valid)]:
            fw.dma("sp", t[:], d, writes=[t])
        fw.op("dve", lambda e: e.tensor_copy(out=identb[:], in_=ident32[:]), reads=[ident32], writes=[identb])
        fw.op("dve", lambda e: e.memset(ones32[:], 1.0 / 1024.0), writes=[ones32])
        fw.op("dve", lambda e: e.memset(onesb[:], 1.0 / 1024.0), writes=[onesb])

        def alloc_ffn_scratch(stk, with_win):
            sc = {}
            sc["xb"] = fw.sb("xb", [128, 8, 512], BF16, stk)
            sc["hT"] = fw.sb("hT", [128, FC, 512], BF16, stk)
            sc["sg"] = [fw.sb("sg%d" % i, [128, 512], BF16, stk) for i in range(2)]
            sc["sq"] = fw.sb("sq", [128, 8, 512], BF16, stk)
            sc["mean"] = fw.sb("mean", [128, 512], F32, stk)
            sc["msq"] = fw.sb("msq", [128, 512], F32, stk)
            sc["rstd"] = fw.sb("rstd", [128, 512], F32, stk)
            sc["wgs"] = [fw.sb("wgs%d" % i, [128, 8, 128], BF16, stk) for i in range(2)]
            sc["wus"] = [fw.sb("wus%d" % i, [128, 8, 128], BF16, stk) for i in range(2)]
            sc["wds"] = [fw.sb("wds%d" % i, [128, FC, 128], BF16, stk) for i in range(2)]
            sc["wcs"] = [fw.sb("wcs%d" % i, [128, 8, 128], BF16, stk) for i in range(3)]
            sc["wci"] = 0
            sc["bki"] = 0
            sc["evi"] = 0
            if with_win:
                sc["wv_sb"] = fw.sb("wv_sb", [128, 8, 512], BF16, stk)
                sc["wwi_sb"] = fw.sb("wwi_sb", [128, 8, 8], BF16, stk)
                fw.dma("sp", sc["wv_sb"][:], wbf["winv"][0], reads=[wtok["winv"][0]], writes=[sc["wv_sb"]])
                fw.dma("sp", sc["wwi_sb"][:], wbf["winwi"][0], reads=[wtok["winwi"][0]], writes=[sc["wwi_sb"]])
                sc["cg_sb"] = fw.sb("cg_sb", [128, 512], F32, stk)
            return sc

        def load_chunk(sc, key, idx):
            s = sc["wcs"][sc["wci"] % 3]
            sc["wci"] += 1
            fw.dma("sp", s[:], wbf[key][idx], reads=[wtok[key][idx]], writes=[s])
            return s

        def nextbank(sc):
            b = bank[sc["bki"] % 6]
            sc["bki"] += 1
            return b

        def ffn(xt, N, kg, ku, kd, sc):
            hT, sg, xb = sc["hT"], sc["sg"], sc["xb"]
            wgs, wus, wds = sc["wgs"], sc["wus"], sc["wds"]

            def load_gu(c):
                fw.dma("sp", wgs[c % 2][:], wbf[kg][c], reads=[wtok[kg][c]], writes=[wgs[c % 2]])
                fw.dma("sp", wus[c % 2][:], wbf[ku][c], reads=[wtok[ku][c]], writes=[wus[c % 2]])

            def load_d(dc):
                fw.dma("sp", wds[dc % 2][:], wbf[kd][dc], reads=[wtok[kd][dc]], writes=[wds[dc % 2]])

            load_gu(0)
            for c in range(FC):
                if c + 1 < FC:
                    load_gu(c + 1)
                else:
                    load_d(0)
                pg, pu = bank[c % 2], bank[2 + c % 2]
                wg_, wu_ = wgs[c % 2], wus[c % 2]
                fw.mm([(lambda e, kc=kc: e.matmul(pg[:, :N], lhsT=wg_[:, kc, :], rhs=xb[:, kc, :N],
                                                  start=(kc == 0), stop=(kc == 7))) for kc in range(8)],
                      reads=[wg_, xb], writes=[pg])
                fw.mm([(lambda e, kc=kc: e.matmul(pu[:, :N], lhsT=wu_[:, kc, :], rhs=xb[:, kc, :N],
                                                  start=(kc == 0), stop=(kc == 7))) for kc in range(8)],
                      reads=[wu_, xb], writes=[pu])
                s_ = sg[c % 2]
                fw.op("act", lambda e: e.activation(out=s_[:, :N], in_=pg[:, :N], func=AF.Silu),
                      reads=[pg], writes=[s_])
                fw.op("dve", lambda e: e.tensor_tensor(out=hT[:, c, :N], in0=s_[:, :N], in1=pu[:, :N], op=ALU.mult),
                      reads=[s_, pu], writes=[hT])
            for dc in range(8):
                if dc + 1 < 8:
                    load_d(dc + 1)
                po = bank[4 + dc % 2]
                wd_ = wds[dc % 2]
                fw.mm([(lambda e, fc=fc: e.matmul(po[:, :N], lhsT=wd_[:, fc, :], rhs=hT[:, fc, :N],
                                                  start=(fc == 0), stop=(fc == FC - 1))) for fc in range(FC)],
                      reads=[wd_, hT], writes=[po])
                fw.op("dve", lambda e: e.scalar_tensor_tensor(out=xt[:, dc, :N], in0=xt[:, dc, :N],
                                                              scalar=2.0 * DN_ALPHA, in1=po[:, :N],
                                                              op0=ALU.mult, op1=ALU.add),
                      reads=[xt, po], writes=[xt])

        def layernorm(xt, N, li, eps, sc):
            sq, mean, msq, rstd, xb = sc["sq"], sc["mean"], sc["msq"], sc["rstd"], sc["xb"]
            fw.op("act", lambda e: e.activation(out=sq[:, :, :N], in_=xt[:, :, :N], func=AF.Square),
                  reads=[xt], writes=[sq])
            p1, p2 = bank[4], bank[5]
            fw.mm([(lambda e, kc=kc: e.matmul(p1[:, :N], lhsT=ones32[:], rhs=xt[:, kc, :N],
                                              start=(kc == 0), stop=(kc == 7))) for kc in range(8)],
                  reads=[ones32, xt], writes=[p1])
            fw.mm([(lambda e, kc=kc: e.matmul(p2[:, :N], lhsT=onesb[:], rhs=sq[:, kc, :N],
                                              start=(kc == 0), stop=(kc == 7))) for kc in range(8)],
                  reads=[onesb, sq], writes=[p2])
            fw.op("act", lambda e: e.copy(out=mean[:, :N], in_=p1[:, :N]), reads=[p1], writes=[mean])
            fw.op("dve", lambda e: e.tensor_tensor(out=msq[:, :N], in0=mean[:, :N], in1=mean[:, :N], op=ALU.mult),
                  reads=[mean], writes=[msq])
            fw.op("dve", lambda e: e.tensor_tensor(out=msq[:, :N], in0=p2[:, :N], in1=msq[:, :N], op=ALU.subtract),
                  reads=[p2, msq], writes=[msq])
            fw.op("dve", lambda e: e.tensor_scalar(out=msq[:, :N], in0=msq[:, :N], scalar1=eps, scalar2=None,
                                                   op0=ALU.add), reads=[msq], writes=[msq])
            fw.op("act", lambda e: e.activation(out=rstd[:, :N], in_=msq[:, :N], func=AF.Sqrt),
                  reads=[msq], writes=[rstd])
            fw.op("dve", lambda e: e.reciprocal(out=rstd[:, :N], in_=rstd[:, :N]), reads=[rstd], writes=[rstd])
            for hf in range(2):
                ks = slice(hf * 4, hf * 4 + 4)
                fw.op("dve", lambda e: e.tensor_tensor(out=xt[:, ks, :N], in0=xt[:, ks, :N], in1=bc_mid(mean[:, :N], 4),
                                                       op=ALU.subtract), reads=[xt, mean], writes=[xt])
                fw.op("dve", lambda e: e.tensor_tensor(out=xt[:, ks, :N], in0=xt[:, ks, :N], in1=bc_mid(rstd[:, :N], 4),
                                                       op=ALU.mult), reads=[xt, rstd], writes=[xt])
                for kc in range(hf * 4, hf * 4 + 4):
                    fw.op("act", lambda e, kc=kc: e.activation(out=xt[:, kc, :N], in_=xt[:, kc, :N], func=AF.Identity,
                                                                scale=lnp[:, (2 * li) * 8 + kc:(2 * li) * 8 + kc + 1],
                                                                bias=lnp[:, (2 * li + 1) * 8 + kc:(2 * li + 1) * 8 + kc + 1]),
                          reads=[xt, lnp], writes=[xt])
            fw.op("dve", lambda e: e.tensor_copy(out=xb[:, :, :N], in_=xt[:, :, :N]), reads=[xt], writes=[xb])

        def proj_st(sc, ci, N, n0=0):
            xb = sc["xb"]
            w_ = load_chunk(sc, "winst", ci)
            b = nextbank(sc)
            fw.mm([(lambda e, kc=kc: e.matmul(b[:, :N], lhsT=w_[:, kc, :], rhs=xb[:, kc, n0:n0 + N],
                                              start=(kc == 0), stop=(kc == 7))) for kc in range(8)],
                  reads=[w_, xb], writes=[b])
            return b

        def evac(sc, out_ap, in_ap, reads, writes, scale=None):
            sc["evi"] += 1
            if sc["evi"] % 2 == 0:
                if scale is None:
                    fw.op("act", lambda e: e.copy(out=out_ap, in_=in_ap), reads=reads, writes=writes)
                else:
                    fw.op("act", lambda e: e.mul(out=out_ap, in_=in_ap, mul=scale), reads=reads, writes=writes)
            else:
                if scale is None:
                    fw.op("dve", lambda e: e.tensor_copy(out=out_ap, in_=in_ap), reads=reads, writes=writes)
                else:
                    fw.op("dve", lambda e: e.tensor_scalar(out=out_ap, in0=in_ap, scalar1=scale, scalar2=None,
                                                           op0=ALU.mult), reads=reads, writes=writes)

        def conv_part(N, sc, own_T):
            cg_sb = sc["cg_sb"]
            for c in range(4):
                pcg = proj_st(sc, CH_CG + c, N)
                pu_ = proj_st(sc, CH_U + c, N)
                fw.op("act", lambda e: e.copy(out=cg_sb[:, :N], in_=pcg[:, :N]), reads=[pcg], writes=[cg_sb])
                if own_T is None:
                    fw.op("dve", lambda e: e.tensor_tensor(out=zh[:, c, :N], in0=cg_sb[:, :N], in1=pu_[:, :N],
                                                           op=ALU.mult), reads=[cg_sb, pu_], writes=[zh])
                    fw.op("dve", lambda e: e.tensor_tensor(out=zh[:, c, :N], in0=zh[:, c, :N], in1=hval[:, :N],
                                                           op=ALU.mult), reads=[zh, hval], writes=[zh])
                    continue
                z, acc = sc["z"], sc["acc"]
                pbg = proj_st(sc, CH_BG + c, N)
                fw.op("dve", lambda e: e.tensor_tensor(out=z[:, :, 2:130],
                                                       in0=cg_sb[:].rearrange("p (b j) -> p b j", b=4),
                                                       in1=pu_[:].rearrange("p (b j) -> p b j", b=4), op=ALU.mult),
                      reads=[cg_sb, pu_], writes=[z])
                fw.op("dve", lambda e: e.tensor_copy(
                    out=z[:, :, 0:2],
                    in_=zh[:, c, own_T * 8:(own_T + 1) * 8].rearrange("p (b k) -> p b k", k=2)),
                      reads=[zh, z], writes=[z])
                fw.op("dve", lambda e: e.tensor_scalar(out=acc[:], in0=z[:, :, 0:128],
                                                       scalar1=convw[:, c * 3 + 0:c * 3 + 1], scalar2=None,
                                                       op0=ALU.mult), reads=[z, convw], writes=[acc])
                for j in (1, 2):
                    fw.op("dve", lambda e, j=j: e.scalar_tensor_tensor(out=acc[:], in0=z[:, :, j:j + 128],
                                                                        scalar=convw[:, c * 3 + j:c * 3 + j + 1],
                                                                        in1=acc[:], op0=ALU.mult, op1=ALU.add),
                          reads=[z, convw, acc], writes=[acc])
                convT = sc["convT"]
                fw.op("dve", lambda e: e.tensor_tensor(out=convT[:, c, :].rearrange("p (b j) -> p b j", b=4),
                                                       in0=acc[:], in1=pbg[:].rearrange("p (b j) -> p b j", b=4),
                                                       op=ALU.mult), reads=[acc, pbg], writes=[convT])

        def kv_part(kt, sc):
            kt_sb, v_sb, xb, wv_sb = sc["kt_sb"], sc["v_sb"], sc["xb"], sc["wv_sb"]
            for j in range(4):
                b = proj_st(sc, CH_K + j, 512)
                evac(sc, kt_sb[:, j, :], b[:], [b], [kt_sb])
            fw.dma("sp", KT_d[kt], kt_sb[:], reads=[kt_sb], writes=[KT_tok[kt]])
            b = proj_st(sc, CH_KI, 512)
            evac(sc, ki2[:, kt * 512:(kt + 1) * 512], b[:], [b], [ki2])
            for blk in range(4):
                b = nextbank(sc)
                fw.mm([(lambda e, kc=kc: e.matmul(b[:], lhsT=xb[:, kc, blk * 128:(blk + 1) * 128], rhs=wv_sb[:, kc, :],
                                                  start=(kc == 0), stop=(kc == 7))) for kc in range(8)],
                      reads=[xb, wv_sb], writes=[b])
                evac(sc, v_sb[:, blk, :], b[:], [b], [v_sb])
            fw.dma("sp", V_d[kt], v_sb[:], reads=[v_sb], writes=[V_tok[kt]])

        def q_part(sc, Tt):
            xb, wwi_sb = sc["xb"], sc["wwi_sb"]
            qc, qic, wis = sc["qc"], sc["qic"], sc["wis"]
            for j in range(4):
                b = proj_st(sc, CH_Q + j, 512)
                evac(sc, qc[:, j, :], b[:], [b], [qc], scale=0.125)
            fw.dma("sp", Q_d[Tt], qc[:], reads=[qc], writes=[Q_tok[Tt]])
            for j in range(4):
                b = proj_st(sc, CH_QI + j, 512)
                evac(sc, qic[:, j, :], b[:], [b], [qic])
            fw.dma("sp", QI_d[Tt], qic[:], reads=[qic], writes=[QI_tok[Tt]])
            b = nextbank(sc)
            for blk in range(4):
                fw.mm([(lambda e, kc=kc: e.matmul(b[:, blk * 8:(blk + 1) * 8], lhsT=xb[:, kc, blk * 128:(blk + 1) * 128],
                                                  rhs=wwi_sb[:, kc, :], start=(kc == 0), stop=(kc == 7)))
                       for kc in range(8)], reads=[xb, wwi_sb], writes=[b])
            fw.op("dve", lambda e: e.tensor_scalar(out=wis[:], in0=b[:, 0:32], scalar1=IDX_SCALE, scalar2=None,
                                                   op0=ALU.mult), reads=[b], writes=[wis])
            fw.dma("sp", WI_d[Tt], wis[:], reads=[wis], writes=[WI_tok[Tt]])

        with fw.scope() as stk:
            sc = alloc_ffn_scratch(stk, True)
            xth = fw.sb("xth", [128, 8, 512], F32, stk)
            fw.dma("sp", xth[:, :, :NH], xT_halo, writes=[xth])
            fw.op("dve", lambda e: e.tensor_copy(out=sc["xb"][:, :, :NH], in_=xth[:, :, :NH]), reads=[xth], writes=[sc["xb"]])
            ffn(xth, NH, "wg1", "wu1", "wd1", sc)
            layernorm(xth, NH, 0, 4.0 * LN_EPS, sc)
            conv_part(NH, sc, None)

        with fw.scope() as stk:
            sc = alloc_ffn_scratch(stk, True)
            sc["z"] = fw.sb("z", [128, 4, 130], F32, stk)
            sc["acc"] = fw.sb("acc", [128, 4, 128], F32, stk)
            sc["kt_sb"] = fw.sb("kt_sb", [128, 4, 512], BF16, stk)
            sc["v_sb"] = fw.sb("v_sb", [128, 4, 512], BF16, stk)
            sc["qc"] = fw.sb("qc", [128, 4, 512], BF16, stk)
            sc["qic"] = fw.sb("qic", [128, 4, 512], BF16, stk)
            sc["wis"] = fw.sb("wis", [128, 32], F32, stk)
            sc["convT"] = fw.sb("convT", [128, 4, 512], BF16, stk)
            xts = [fw.sb("xts%d" % i, [128, 8, 512], F32, stk) for i in range(2)]
            xb = sc["xb"]
            for Tt in range(NT):
                for sub in (0, 1):
                    xt = xts[sub]
                    src = xT_own if sub == 0 else xT_oth
                    fw.dma("sp", xt[:], src[Tt], writes=[xt])
                    fw.op("dve", lambda e: e.tensor_copy(out=xb[:], in_=xt[:]), reads=[xt], writes=[xb])
                    ffn(xt, 512, "wg1", "wu1", "wd1", sc)
                    layernorm(xt, 512, 0, 4.0 * LN_EPS, sc)
                    if sub == 0:
                        fw.dma("sp", X1_d[Tt], xt[:], reads=[xt], writes=[X1_tok[Tt]])
                    kv_part(2 * Tt + sub, sc)
                    if sub == 0:
                        q_part(sc, Tt)
                        conv_part(512, sc, Tt)
                        fw.dma("sp", CONV_d[Tt], sc["convT"][:], reads=[sc["convT"]], writes=[CONV_tok[Tt]])

        with fw.scope() as stk:
            score = [fw.sb("score%d" % i, [128, NKT * 512], F32, stk) for i in range(2)]
            junk = fw.sb("junk", [128, NKT * 512], BF16, stk)
            Rr = [fw.sb("R%d" % i, [128, 512], BF16, stk) for i in range(3)]
            Tts = [fw.sb("Tt%d" % i, [128, 512], F32, stk) for i in range(3)]
            Pp = [fw.sb("P%d" % i, [128, 512], BF16, stk) for i in range(3)]
            PTs = [fw.sb("PT%d" % i, [128, 4, 128], BF16, stk) for i in range(3)]
            ktile = [fw.sb("ktile%d" % i, [128, 4, 512], BF16, stk) for i in range(2)]
            vtile = [fw.sb("vtile%d" % i, [128, 4, 512], BF16, stk) for i in range(2)]
            diag = fw.sb("diag", [128, 8, 128], BF16, stk)
            distn = [fw.sb("distn%d" % i, [128, 512], F32, stk) for i in range(2)]
            mb = fw.sb("mb", [128, 512], F32, stk)
            attn_sb = fw.sb("attn_sb", [128, 512], BF16, stk)
            amaxc = [fw.sb("amaxc%d" % i, [128, NKT], F32, stk) for i in range(2)]
            small = [fw.sb("small%d" % i, [128, 16], F32, stk) for i in range(2)]
            Wi = [fw.sb("Wi%d" % i, [128, NIT], F32, stk) for i in range(2)]
            cnt = [fw.sb("cnt%d" % i, [128, NIT * 4], F32, stk) for i in range(2)]
            rowsum = fw.sb("rowsum", [128, 8 * NKT], F32, stk)
            rs = fw.sb("rs", [128, 8], F32, stk)
            biasc = fw.sb("biasc", [128, 8], F32, stk)
            qXs = [[fw.sb("qX%d_%d" % (s_, i), [128, 4, 512], BF16, stk) for i in range(2)] for s_ in range(2)]
            qiXs = [[fw.sb("qiX%d_%d" % (s_, i), [128, 4, 512], BF16, stk) for i in range(2)] for s_ in range(2)]
            wi_sbs = [fw.sb("wi_sb%d" % s_, [128, 32], F32, stk) for s_ in range(2)]
            attT = [fw.sb("attT%d" % s_, [128, 4, 512], BF16, stk) for s_ in range(2)]
            for s_ in range(2):
                for t in qXs[s_] + qiXs[s_]:
                    fw.op("dve", lambda e, t=t: e.memset(t[:], 0.0), writes=[t])

            def load_tile_q(Tt):
                s_ = Tt % 2
                for eh in range(2):
                    ps = slice(eh * 64, (eh + 1) * 64)
                    fw.dma("sp", qXs[s_][eh][ps], Q_d[Tt][ps], reads=[Q_tok[Tt]], writes=[qXs[s_][eh]])
                    fw.dma("sp", qiXs[s_][eh][ps], QI_d[Tt][ps], reads=[QI_tok[Tt]], writes=[qiXs[s_][eh]])
                fw.dma("sp", wi_sbs[s_][:], WI_d[Tt], reads=[WI_tok[Tt]], writes=[wi_sbs[s_]])

            def gen_indexer(g):
                Tt, qb = divmod(g, 4)
                nkt = 2 * Tt + 2
                if qb == 0:
                    load_tile_q(Tt)
                par = g % 2
                qiX, wi_sb = qiXs[Tt % 2], wi_sbs[Tt % 2]
                qs = slice(qb * 128, (qb + 1) * 128)
                sco, amx = score[par], amaxc[par]
                for h in range(8):
                    fw.op("dve", lambda e, h=h: e.tensor_scalar(out=diag[:, h, :], in0=identb[:],
                                                                 scalar1=wi_sb[:, qb * 8 + h:qb * 8 + h + 1],
                                                                 scalar2=None, op0=ALU.mult),
                          reads=[identb, wi_sb], writes=[diag])
                items = [(kt, h) for kt in range(nkt) for h in range(8)]
                n = len(items)

                def A(i):
                    kt, h = items[i]
                    hp, eh = divmod(h, 2)
                    pr = bank[i % 3]
                    fw.mm([lambda e: e.matmul(pr[:], lhsT=qiX[eh][:, hp, qs], rhs=ki2[:, kt * 512:(kt + 1) * 512],
                                              start=True, stop=True)], reads=[qiX[eh], ki2], writes=[pr])

                def B(i):
                    pr, r_ = bank[i % 3], Rr[i % 3]
                    if items[i][1] < 5:
                        fw.op("act", lambda e: e.activation(out=r_[:], in_=pr[:], func=AF.Relu), reads=[pr], writes=[r_])
                    else:
                        fw.op("dve", lambda e: e.tensor_scalar(out=r_[:], in0=pr[:], scalar1=0.0, scalar2=None,
                                                               op0=ALU.max), reads=[pr], writes=[r_])

                def C(i):
                    kt, h = items[i]
                    psc, r_ = bank[3 + kt % 2], Rr[i % 3]
                    fw.mm([lambda e: e.matmul(psc[:], lhsT=diag[:, h, :], rhs=r_[:], start=(h == 0), stop=(h == 7))],
                          reads=[diag, r_], writes=[psc])
                    if h == 7:
                        sl = slice(kt * 512, (kt + 1) * 512)
                        fw.op("dve", lambda e: e.tensor_copy(out=sco[:, sl], in_=psc[:]), reads=[psc], writes=[sco])
                        fw.op("dve", lambda e: e.tensor_reduce(out=amx[:, kt:kt + 1], in_=sco[:, sl], axis=AX.X,
                                                               op=ALU.max, apply_absolute_value=True),
                              reads=[sco], writes=[amx])
                        if kt >= 2 * Tt:
                            sub = kt - 2 * Tt
                            fw.op("dve", lambda e: e.tensor_scalar(out=mb[:], in0=offrows[:, sub, :],
                                                                   scalar1=limk[:, g:g + 1], scalar2=NEG,
                                                                   op0=ALU.is_ge, op1=ALU.mult),
                                  reads=[offrows, limk], writes=[mb])
                            fw.op("dve", lambda e: e.tensor_tensor(out=sco[:, sl], in0=sco[:, sl], in1=mb[:],
                                                                   op=ALU.add), reads=[sco, mb], writes=[sco])
                A(0)
                A(1)
                for i in range(n):
                    if i + 2 < n:
                        A(i + 2)
                    B(i)
                    C(i)
                    if items[i][1] == 7:
                        yield

            def gen_bisect(g):
                Tt, qb = divmod(g, 4)
                nkt = 2 * Tt + 2
                L = nkt * 512
                par = g % 2
                sco, amx, sm, wi_, cn = score[par], amaxc[par], small[par], Wi[par], cnt[par]
                fw.op("dve", lambda e: e.memset(cn[:], 0.0), writes=[cn])
                fw.op("dve", lambda e: e.tensor_reduce(out=sm[:, 0:1], in_=amx[:, :nkt], axis=AX.X, op=ALU.max),
                      reads=[amx], writes=[sm])
                fw.op("dve", lambda e: e.tensor_scalar(out=sm[:, 1:2], in0=sm[:, 0:1], scalar1=2.02,
                                                       scalar2=2e-6, op0=ALU.mult, op1=ALU.add),
                      reads=[sm], writes=[sm])
                fw.op("dve", lambda e: e.tensor_scalar(out=wi_[:], in0=pow2[:], scalar1=sm[:, 1:2], scalar2=None,
                                                       op0=ALU.mult), reads=[pow2, sm], writes=[wi_])
                fw.op("dve", lambda e: e.memset(sm[:, 2:3], 0.0), reads=[sm], writes=[sm])
                CH = 2048
                nch = (L + CH - 1) // CH
                for it in range(NIT):
                    for c in range(nch):
                        c0, c1 = c * CH, min(L, (c + 1) * CH)
                        fw.op("dve", lambda e: e.tensor_scalar(out=junk[:, c0:c1], in0=sco[:, c0:c1],
                                                               scalar1=sm[:, 2:3], scalar2=0.0,
                                                               op0=ALU.is_ge, op1=ALU.add,
                                                               accum_out=cn[:, it * 4 + c:it * 4 + c + 1]),
                              reads=[sco, sm, cn], writes=[junk, cn])
                        if c + 1 < nch:
                            yield
                    if nch > 1:
                        fw.op("dve", lambda e: e.tensor_reduce(out=sm[:, 6:7], in_=cn[:, it * 4:it * 4 + nch], axis=AX.X,
                                                               op=ALU.add), reads=[cn, sm], writes=[sm])
                        csrc = sm[:, 6:7]
                    else:
                        csrc = cn[:, it * 4:it * 4 + 1]
                    fw.op("dve", lambda e: e.tensor_scalar(out=sm[:, 3:4], in0=csrc,
                                                           scalar1=float(TOPK), scalar2=-0.5,
                                                           op0=ALU.is_ge, op1=ALU.add),
                          reads=[cn, sm], writes=[sm])
                    fw.op("dve", lambda e: e.scalar_tensor_tensor(out=sm[:, 2:3], in0=sm[:, 3:4],
                                                                  scalar=wi_[:, it:it + 1], in1=sm[:, 2:3],
                                                                  op0=ALU.mult, op1=ALU.add),
                          reads=[sm, wi_], writes=[sm])
                    yield
                fw.op("dve", lambda e: e.scalar_tensor_tensor(out=sm[:, 4:5], in0=wi_[:, NIT - 1:NIT], scalar=-0.5,
                                                              in1=sm[:, 2:3], op0=ALU.mult, op1=ALU.add),
                      reads=[sm, wi_], writes=[sm])

            def gen_estep(g):
                Tt, qb = divmod(g, 4)
                nkt = 2 * Tt + 2
                L = nkt * 512
                par = g % 2
                sco, sm = score[par], small[par]
                for kt in range(nkt):
                    Tk, sub = divmod(kt, 2)
                    dn = distn[kt % 2]
                    sl = slice(kt * 512, (kt + 1) * 512)
                    fw.op("act", lambda e: e.activation(out=dn[:], in_=offrows[:, sub, :], func=AF.Abs,
                                                        bias=tqk[:, g * NT + Tk:g * NT + Tk + 1], scale=1.0),
                          reads=[offrows, tqk], writes=[dn])
                    fw.op("dve", lambda e: e.scalar_tensor_tensor(out=sco[:, sl], in0=sco[:, sl],
                                                                  scalar=sm[:, 4:5], in1=dn[:],
                                                                  op0=ALU.is_lt, op1=ALU.add),
                          reads=[sco, sm, dn], writes=[sco])
                    yield
                fw.op("dve", lambda e: e.tensor_reduce(out=sm[:, 5:6], in_=sco[:, :L], axis=AX.X, op=ALU.min),
                      reads=[sco], writes=[sm])
                fw.op("dve", lambda e: e.tensor_scalar(out=biasc[:], in0=slopebig[:], scalar1=sm[:, 5:6],
                                                       scalar2=None, op0=ALU.mult),
                      reads=[slopebig, sm], writes=[biasc])

            def gen_attn(g):
                Tt, qb = divmod(g, 4)
                nkt = 2 * Tt + 2
                par = g % 2
                qX = qXs[Tt % 2]
                at_ = attT[Tt % 2]
                qs = slice(qb * 128, (qb + 1) * 128)
                sco = score[par]
                fw.op("dve", lambda e: e.memset(rowsum[:], 0.0), writes=[rowsum])
                items = [(kt, h) for kt in range(nkt) for h in range(8)]
                n = len(items)

                def load_kv(kt):
                    fw.dma("sp", ktile[kt % 2][:], KT_d[kt], reads=[KT_tok[kt]], writes=[ktile[kt % 2]])
                    fw.dma("sp", vtile[kt % 2][:], V_d[kt], reads=[V_tok[kt]], writes=[vtile[kt % 2]])

                def A(i):
                    kt, h = items[i]
                    hp, eh = divmod(h, 2)
                    ps_, kt_ = bank[i % 3], ktile[kt % 2]
                    fw.mm([lambda e: e.matmul(ps_[:], lhsT=qX[eh][:, hp, qs], rhs=kt_[:, hp, :], start=True, stop=True)],
                          reads=[qX[eh], kt_], writes=[ps_])

                def BC(i):
                    kt, h = items[i]
                    sl = slice(kt * 512, (kt + 1) * 512)
                    ps_, t_, p_ = bank[i % 3], Tts[i % 3], Pp[i % 3]
                    fw.op("dve", lambda e: e.scalar_tensor_tensor(out=t_[:], in0=sco[:, sl], scalar=-SLOPES[h] * BIG,
                                                                  in1=ps_[:], op0=ALU.mult, op1=ALU.add),
                          reads=[sco, ps_], writes=[t_])
                    fw.op("act", lambda e: e.activation(out=p_[:], in_=t_[:], func=AF.Exp, bias=biasc[:, h:h + 1], scale=1.0,
                                                        accum_out=rowsum[:, h * NKT + kt:h * NKT + kt + 1]),
                          reads=[t_, biasc, rowsum], writes=[p_])

                def DE(i):
                    p_, pt_ = Pp[i % 3], PTs[i % 3]
                    hb = i % 2
                    pbf = pbfs[hb]
                    fw.mm([(lambda e, sbk=sbk: e.transpose(out=pbf[:, hb, sbk * 128:(sbk + 1) * 128],
                                                           in_=p_[:, sbk * 128:(sbk + 1) * 128], identity=identb[:]))
                           for sbk in range(4)], reads=[p_, identb], writes=[pbf])
                    src_ap = pbf[:, hb, :].rearrange("p (s q) -> p s q", s=4)
                    fw.op("act", lambda e: e.copy(out=pt_[:], in_=src_ap), reads=[pbf], writes=[pt_])

                def Fm(i):
                    kt, h = items[i]
                    pt_, vt_ = PTs[i % 3], vtile[kt % 2]
                    fw.mm([(lambda e, sbk=sbk: e.matmul(PO[:, h * 64:(h + 1) * 64], lhsT=pt_[:, sbk, :],
                                                        rhs=vt_[:, sbk, h * 64:(h + 1) * 64],
                                                        start=(i == 0 and sbk == 0), stop=(i == n - 1 and sbk == 3)))
                           for sbk in range(4)], reads=[pt_, vt_], writes=[PO])
                load_kv(0)
                if nkt > 1:
                    load_kv(1)
                A(0)
                A(1)
                BC(0)
                for i in range(n):
                    if i + 2 < n:
                        A(i + 2)
                    if i + 1 < n:
                        BC(i + 1)
                    DE(i)
                    if i >= 1:
                        Fm(i - 1)
                        kt_prev, h_prev = items[i - 1]
                        if h_prev == 7 and kt_prev + 2 < nkt:
                            load_kv(kt_prev + 2)
                    yield
                Fm(n - 1)
                fw.op("dve", lambda e: e.tensor_reduce(out=rs[:], in_=rowsum[:].rearrange("p (h k) -> p h k", k=NKT),
                                                       axis=AX.X, op=ALU.add), reads=[rowsum], writes=[rs, rowsum])
                fw.op("dve", lambda e: e.reciprocal(out=rs[:], in_=rs[:]), reads=[rs], writes=[rs])
                fw.op("dve", lambda e: e.tensor_tensor(
                    out=attn_sb[:].rearrange("p (h d) -> p h d", d=64),
                    in0=PO[:].rearrange("p (h d) -> p h d", d=64),
                    in1=rs[:].rearrange("p (h o) -> p h o", o=1).to_broadcast([128, 8, 64]), op=ALU.mult),
                      reads=[PO, rs], writes=[attn_sb])
                pbf = pbfs[0]
                fw.mm([(lambda e, c=c: e.transpose(out=pbf[:, 0, c * 128:(c + 1) * 128],
                                                   in_=attn_sb[:, c * 128:(c + 1) * 128], identity=identb[:]))
                       for c in range(4)], reads=[attn_sb, identb], writes=[pbf])
                fw.op("act", lambda e: e.copy(out=at_[:, :, qs],
                                              in_=pbf[:, 0, :].rearrange("p (c q) -> p c q", c=4)),
                      reads=[pbf], writes=[at_])
                if qb == 3:
                    fw.dma("sp", ATT_d[Tt], at_[:], reads=[at_], writes=[ATT_tok[Tt]])

            def run_pair(ga, na, gb, nb):
                a_alive, b_alive = ga is not None, gb is not None
                ia = ib = 0
                while a_alive or b_alive:
                    pa = (ia / na) if a_alive else 2.0
                    pb = (ib / nb) if b_alive else 2.0
                    if pa <= pb:
                        try:
                            next(ga)
                            ia += 1
                        except StopIteration:
                            a_alive = False
                    else:
                        try:
                            next(gb)
                            ib += 1
                        except StopIteration:
                            b_alive = False

            def nkt_of(g):
                return 2 * (g // 4) + 2

            for _ in gen_indexer(0):
                pass
            for g in range(NQB):
                run_pair(gen_bisect(g), NIT * ((nkt_of(g) * 512 + 2047) // 2048), gen_attn(g - 1) if g >= 1 else None,
                         8 * nkt_of(max(g - 1, 0)))
                run_pair(gen_indexer(g + 1) if g + 1 < NQB else None, nkt_of(min(g + 1, NQB - 1)),
                         gen_estep(g), nkt_of(g))
            for _ in gen_attn(NQB - 1):
                pass

        with fw.scope() as stk:
            sc = alloc_ffn_scratch(stk, False)
            xb = sc["xb"]
            x1s = [fw.sb("x1s%d" % i, [128, 8, 512], F32, stk) for i in range(2)]
            cats = [fw.sb("cats%d" % i, [128, 8, 512], BF16, stk) for i in range(2)]
            pt32s = [fw.sb("pt32_%d" % i, [128, 2, 512], F32, stk) for i in range(2)]
            pb = fw.sb("pb", [128, 2, 512], BF16, stk)
            sgm = [fw.sb("sgm%d" % i, [128, 512], F32, stk) for i in range(2)]
            wps = [fw.sb("wps%d" % i, [128, 2, 128], BF16, stk) for i in range(2)]

            def p3_loads(Tt):
                s_ = Tt % 2
                fw.dma("sp", x1s[s_][:], X1_d[Tt], reads=[X1_tok[Tt]], writes=[x1s[s_]])
                fw.dma("sp", cats[s_][:, 0:4, :], ATT_d[Tt], reads=[ATT_tok[Tt]], writes=[cats[s_]])
                fw.dma("sp", cats[s_][:, 4:8, :], CONV_d[Tt], reads=[CONV_tok[Tt]], writes=[cats[s_]])
                fw.dma("sp", pt32s[s_][:], pT_own[Tt], writes=[pt32s[s_]])
            p3_loads(0)
            for Tt in range(NT):
                if Tt + 1 < NT:
                    p3_loads(Tt + 1)
                x1o, catT, pt32 = x1s[Tt % 2], cats[Tt % 2], pt32s[Tt % 2]
                fw.op("dve", lambda e: e.tensor_copy(out=pb[:], in_=pt32[:]), reads=[pt32], writes=[pb])
                for dc in range(8):
                    w_ = load_chunk(sc, "wout", dc)
                    b = nextbank(sc)
                    fw.mm([(lambda e, fc=fc: e.matmul(b[:], lhsT=w_[:, fc, :], rhs=catT[:, fc, :],
                                                      start=(fc == 0), stop=(fc == 7))) for fc in range(8)],
                          reads=[w_, catT], writes=[b])
                    fw.op("dve", lambda e: e.scalar_tensor_tensor(out=x1o[:, dc, :], in0=x1o[:, dc, :], scalar=DN_ALPHA,
                                                                  in1=b[:], op0=ALU.mult, op1=ALU.add),
                          reads=[x1o, b], writes=[x1o])
                layernorm(x1o, 512, 1, LN_EPS, sc)
                ffn(x1o, 512, "wg2", "wu2", "wd2", sc)
                layernorm(x1o, 512, 2, 4.0 * LN_EPS, sc)
                for dc in range(8):
                    w_ = load_chunk(sc, "wgate", dc)
                    wp_ = wps[dc % 2]
                    sg_ = sgm[dc % 2]
                    fw.dma("sp", wp_[:], wbf["wproj"][dc], reads=[wtok["wproj"][dc]], writes=[wp_])
                    bg_ = nextbank(sc)
                    bp_ = nextbank(sc)
                    fw.mm([(lambda e, fc=fc: e.matmul(bg_[:], lhsT=w_[:, fc, :], rhs=xb[:, fc, :],
                                                      start=(fc == 0), stop=(fc == 7))) for fc in range(8)],
                          reads=[w_, xb], writes=[bg_])
                    fw.mm([(lambda e, fc=fc: e.matmul(bp_[:], lhsT=wp_[:, fc, :], rhs=pb[:, fc, :],
                                                      start=(fc == 0), stop=(fc == 1))) for fc in range(2)],
                          reads=[wp_, pb], writes=[bp_])
                    fw.op("act", lambda e: e.activation(out=sg_[:], in_=bg_[:], func=AF.Sigmoid), reads=[bg_], writes=[sg_])
                    fw.op("dve", lambda e: e.tensor_tensor(out=sg_[:], in0=sg_[:], in1=bp_[:], op=ALU.mult),
                          reads=[sg_, bp_], writes=[sg_])
                    fw.op("dve", lambda e: e.tensor_tensor(out=x1o[:, dc, :], in0=x1o[:, dc, :], in1=sg_[:], op=ALU.add),
                          reads=[x1o, sg_], writes=[x1o])
                fw.dma("sp", outT[Tt], x1o[:], reads=[x1o], writes=[out_tok])
        fw.barrier()
    return nc, fw


def _tile_w(W, kc, cc):
    return np.ascontiguousarray(W.reshape(kc, 128, cc, 128).transpose(2, 1, 0, 3))


def prep_weights(inp):
    f = np.float32
    w = {}
    w["wg1"] = _tile_w(inp["ffn1_wg"][0], 8, FC)
    w["wu1"] = _tile_w(inp["ffn1_wu"][0], 8, FC)
    w["wd1"] = _tile_w(inp["ffn1_wd"][0], FC, 8)
    w["wg2"] = _tile_w(inp["ffn2_wg"][0], 8, FC)
    w["wu2"] = _tile_w(inp["ffn2_wu"][0], 8, FC)
    w["wd2"] = _tile_w(inp["ffn2_wd"][0], FC, 8)
    win = inp["w_in"][0]
    cols = {"q": (0, 512), "k": (512, 1024), "v": (1024, 1536), "qi": (1536, 2048), "ki": (2048, 2112),
            "wi": (2112, 2120), "bg": (2120, 2632), "cg": (2632, 3144), "u": (3144, 3656)}

    def sub(n):
        a, b = cols[n]
        return win[:, a:b]
    st = np.concatenate([sub("k"), sub("ki"), sub("ki"), sub("q"), sub("qi"), sub("bg"), sub("cg"), sub("u")], axis=1)
    assert st.shape[1] == NCH * 128
    w["winst"] = _tile_w(st, 8, NCH)
    w["winv"] = np.ascontiguousarray(sub("v").reshape(8, 128, 512).transpose(1, 0, 2))[None]
    w["winwi"] = np.ascontiguousarray(sub("wi").reshape(8, 128, 8).transpose(1, 0, 2))[None]
    w["wout"] = _tile_w(inp["w_out"][0], 8, 8)
    w["wgate"] = _tile_w(inp["ple_gate_w"][0], 8, 8)
    w["wproj"] = _tile_w(inp["ple_proj_w"][0], 2, 8)
    lnp = np.stack([inp["ln1_g"][0], inp["ln1_b"][0], inp["ln2_g"][0], inp["ln2_b"][0],
                    inp["ln3_g"][0], inp["ln3_b"][0]], 0)
    w["lnp"] = np.ascontiguousarray(lnp.reshape(6, 8, 128).transpose(2, 0, 1).reshape(128, 48))
    cw = inp["conv_w"][0]
    w["convw"] = np.ascontiguousarray(cw.reshape(3, 4, 128).transpose(2, 1, 0).reshape(128, 12))
    w["ident"] = np.eye(128, dtype=f)
    w["slopebig"] = np.tile(np.array([s * BIG for s in SLOPES], dtype=f)[None, :], (128, 1))
    w["pow2"] = np.tile(np.array([2.0 ** (-(i + 1)) for i in range(NIT)], dtype=f)[None, :], (128, 1))
    return {k: np.ascontiguousarray(v, dtype=f) for k, v in w.items()}


def core_blocks(NT, role):
    own, oth = [], []
    for t in range(NT):
        b = 8 * t
        a = [b, b + 3, b + 4, b + 7]
        o = [b + 1, b + 2, b + 5, b + 6]
        if role == 0:
            own.append(a); oth.append(o)
        else:
            own.append(o); oth.append(a)
    return own, oth


def prep_core(x_b, p_b, NT, role):
    f = np.float32
    own, oth = core_blocks(NT, role)
    ar = np.arange(128)

    def pos_of(blocks):
        return np.concatenate([b * 128 + ar for b in blocks])
    own_pos = [pos_of(b) for b in own]
    oth_pos = [pos_of(b) for b in oth]

    def featmajor(rows, kc):
        return np.ascontiguousarray(rows.T.reshape(kc, 128, rows.shape[0]).transpose(1, 0, 2))
    m = {}
    m["xT_own"] = np.stack([featmajor(x_b[p], 8) for p in own_pos], 0)
    m["xT_oth"] = np.stack([featmajor(x_b[p], 8) for p in oth_pos], 0)
    m["pT_own"] = np.stack([featmajor(p_b[p], 2) for p in own_pos], 0)
    hpos, hval = [], []
    for t in range(NT):
        for b in own[t]:
            s = b * 128
            if s == 0:
                hpos += [0, 1]; hval += [0.0, 0.0]
            else:
                hpos += [s - 2, s - 1]; hval += [1.0, 1.0]
    m["xT_halo"] = featmajor(x_b[np.array(hpos)], 8)
    m["halo_valid"] = np.tile(np.array(hval, dtype=f)[None, :], (128, 1))
    off = np.stack([own_pos[0], oth_pos[0]], 0).astype(np.float64)
    m["offrows"] = np.tile((off * PSC)[None], (128, 1, 1))
    NQB = NT * 4
    tqk = np.zeros((128, NQB, NT), dtype=np.float64)
    limk = np.zeros((128, NQB), dtype=np.float64)
    for t in range(NT):
        for qb in range(4):
            pq = own_pos[t][qb * 128:(qb + 1) * 128].astype(np.float64)
            for tk in range(NT):
                tqk[:, t * 4 + qb, tk] = -(pq - 1024.0 * tk) * PSC
            lim = (np.floor(pq / 64) + 1) * 64
            limk[:, t * 4 + qb] = (lim - 1024.0 * t) * PSC
    m["tqk"] = tqk.reshape(128, NQB * NT)
    m["limk"] = limk
    return {k: np.ascontiguousarray(v, dtype=f) for k, v in m.items()}, own_pos


_CACHE = {}


def run_module(inputs, NT, B):
    x = np.asarray(inputs["x"], dtype=np.float32)
    p = np.asarray(inputs["p"], dtype=np.float32)[0]
    wts = prep_weights({k: np.asarray(v, dtype=np.float32) for k, v in inputs.items() if k not in ("x", "p")})
    in_maps, poss = [], []
    for b in range(B):
        for role in (0, 1):
            m, own_pos = prep_core(x[b], p[b], NT, role)
            m.update(wts)
            in_maps.append(m)
            poss.append((b, own_pos))
    if NT not in _CACHE:
        _CACHE[NT] = build_program(NT)
    nc, fw = _CACHE[NT]
    import time as _t
    _t0 = _t.time()
    res = run_bass_kernel_spmd(nc, in_maps, core_ids=list(range(2 * B)), trace=bool(os.environ.get('KTRACE')))
    if os.environ.get('KTRACE'):
        print('exec_time_ns', res.exec_time_ns, flush=True)
    if os.environ.get('KVERB'):
        print("ninstr", fw.ninstr, "spmd run s", _t.time() - _t0, flush=True)
    if os.environ.get('KRAW'):
        return [np.asarray(r["outT"]) for r in res.results]
    out = np.zeros((B, NT * 1024, D), dtype=np.float32)
    for ci, (b, own_pos) in enumerate(poss):
        oT = np.asarray(res.results[ci]["outT"])
        for t in range(NT):
            rows = oT[t].transpose(2, 1, 0).reshape(512, D)
            out[b, own_pos[t]] = rows
    return out


def kernel(**inputs):
    return run_module(inputs, 8, 4)
```
